# Optimizing a Trainium2 kernel written in Bass

```python
import jax, jax.numpy as jnp
from jax import lax
import numpy as np

D_MODEL = 2048
BATCH = 2
SEQ = 4096
DEPTH = 1

GRID_W = 64
CTX_LEN = 256
HEAD_DIM = 128
N_HEADS = D_MODEL // HEAD_DIM
N_KV_HEADS = N_HEADS // 4
Q_GROUP = N_HEADS // N_KV_HEADS
ATT_WIDTH = N_HEADS * HEAD_DIM
KV_WIDTH = N_KV_HEADS * HEAD_DIM
ROPE_FREQS = HEAD_DIM // 4
ROPE_THETA = 10000.0
Q_BLOCK = 128
F_GROUP_DIM = 128
F_WIDTH = D_MODEL // 2
F_GROUPS = F_WIDTH // F_GROUP_DIM
N_EXPERTS = 64
N_EXPERT_GROUPS = 8
TOPK_GROUPS = 4
TOP_K = 8
EXPERT_FF = D_MODEL // 4
SHARED_FF = D_MODEL // 4
ROUTED_SCALE = 2.5
EXPERT_BLOCK = 128
EPS = 1e-6
Q_OFF = F_WIDTH
K_OFF = Q_OFF + ATT_WIDTH
V_OFF = K_OFF + KV_WIDTH
GF_OFF = V_OFF + KV_WIDTH
GA_OFF = GF_OFF + D_MODEL
IN_COLS = GA_OFF + D_MODEL

kernel_name = 'hybrid_fourier_gqa_moe_dit_block'


def rmsnorm(x, w):
    x32 = x.astype(jnp.float32)
    y = x32 * lax.rsqrt(jnp.mean(x32 * x32, axis=-1, keepdims=True) + EPS)
    return (y * w.astype(jnp.float32)).astype(x.dtype)


def modulate(h, shift, scale):
    return h * (1 + scale) + shift


def axial_rope(n_tokens):
    rows = n_tokens // GRID_W
    row = jnp.repeat(jnp.arange(rows), GRID_W)
    col = jnp.tile(jnp.arange(GRID_W), rows)
    pos = jnp.stack([row, col], axis=-1).astype(jnp.float32)
    inv_freq = ROPE_THETA ** (-jnp.arange(ROPE_FREQS, dtype=jnp.float32) / ROPE_FREQS)
    ang = pos[:, :, None] * inv_freq
    return jnp.cos(ang), jnp.sin(ang)


def apply_rope(x, cos, sin):
    b, s, h, _ = x.shape
    xr = x.reshape(b, s, h, 2, 2, ROPE_FREQS)
    xa, xb = xr[..., 0, :], xr[..., 1, :]
    c = cos[None, :, None].astype(x.dtype)
    sn = sin[None, :, None].astype(x.dtype)
    out = jnp.stack([xa * c - xb * sn, xb * c + xa * sn], axis=-2)
    return out.reshape(b, s, h, HEAD_DIM)


def to_heads(t, n_heads):
    return t.reshape(t.shape[0], t.shape[1], n_heads, HEAD_DIM)


def attend(qb, k, v):
    s = jnp.einsum('bqkgd,bskd->bkgqs', qb, k).astype(jnp.float32) * (HEAD_DIM ** -0.5)
    p = jax.nn.softmax(s, axis=-1).astype(v.dtype)
    return jnp.einsum('bkgqs,bskd->bqkgd', p, v)


def latent_attention(q, k, v, kc, vc):
    b, s = q.shape[:2]
    k_all = jnp.concatenate([k, kc], axis=1)
    v_all = jnp.concatenate([v, vc], axis=1)
    qb = q.reshape(b, s // Q_BLOCK, Q_BLOCK, N_KV_HEADS, Q_GROUP, HEAD_DIM).transpose(1, 0, 2, 3, 4, 5)
    o = lax.map(lambda qi: attend(qi, k_all, v_all), qb)
    return o.transpose(1, 0, 2, 3, 4, 5).reshape(b, s, ATT_WIDTH)


def context_attention(qc, kc, vc):
    b, n = qc.shape[:2]
    return attend(qc.reshape(b, n, N_KV_HEADS, Q_GROUP, HEAD_DIM), kc, vc).reshape(b, n, ATT_WIDTH)


def fourier_mix(u):
    b, n, _ = u.shape
    ug = u.astype(jnp.float32).reshape(b, n, F_GROUPS, F_GROUP_DIM)
    y = jnp.fft.fft2(ug, axes=(1, 3), norm='ortho').real
    return y.reshape(b, n, F_WIDTH).astype(u.dtype)


def split_projection(p):
    return (p[..., :Q_OFF], p[..., Q_OFF:K_OFF], p[..., K_OFF:V_OFF],
            p[..., V_OFF:GF_OFF], p[..., GF_OFF:GA_OFF], p[..., GA_OFF:])


def merge_branches(yf, ya, gf, ga, w_fo, w_ao, w_o):
    y = jax.nn.sigmoid(gf) * (yf @ w_fo) + jax.nn.sigmoid(ga) * (ya @ w_ao)
    return y @ w_o


def swiglu(x, wg, wu, wd):
    return (jax.nn.silu(x @ wg) * (x @ wu)) @ wd


def route(h, router_w, router_b):
    scores = jax.nn.sigmoid(h.astype(jnp.float32) @ router_w.astype(jnp.float32))
    biased = scores + router_b.astype(jnp.float32)
    grp = biased.reshape(-1, N_EXPERT_GROUPS, N_EXPERTS // N_EXPERT_GROUPS)
    gscore = lax.top_k(grp, 2)[0].sum(-1)
    _, gidx = lax.top_k(gscore, TOPK_GROUPS)
    gmask = jax.nn.one_hot(gidx, N_EXPERT_GROUPS, dtype=jnp.float32).sum(1) > 0
    emask = jnp.repeat(gmask, N_EXPERTS // N_EXPERT_GROUPS, axis=1)
    _, eidx = lax.top_k(jnp.where(emask, biased, -jnp.inf), TOP_K)
    w = jnp.take_along_axis(scores, eidx, axis=1)
    w = w / jnp.sum(w, axis=-1, keepdims=True) * ROUTED_SCALE
    return eidx, w


def moe(h, router_w, router_b, exp_gate, exp_up, exp_down, sh_gate, sh_up, sh_down):
    t = h.shape[0]
    eidx, w = route(h, router_w, router_b)
    n_assign = t * TOP_K
    flat_e = eidx.reshape(-1)
    flat_tok = jnp.repeat(jnp.arange(t, dtype=jnp.int32), TOP_K)
    flat_w = w.reshape(-1).astype(h.dtype)
    order = jnp.argsort(flat_e)
    se, stok, sw = flat_e[order], flat_tok[order], flat_w[order]
    counts = jnp.bincount(flat_e, length=N_EXPERTS)
    starts = jnp.cumsum(counts) - counts
    padded = (counts + EXPERT_BLOCK - 1) // EXPERT_BLOCK * EXPERT_BLOCK
    pends = jnp.cumsum(padded)
    pstarts = pends - padded
    dest = pstarts[se] + (jnp.arange(n_assign) - starts[se])
    n_blocks = -(-n_assign // EXPERT_BLOCK) + N_EXPERTS
    n_slots = n_blocks * EXPERT_BLOCK
    slot_tok = jnp.full((n_slots,), t, jnp.int32).at[dest].set(stok)
    slot_w = jnp.zeros((n_slots,), h.dtype).at[dest].set(sw)
    block_e = jnp.minimum(jnp.searchsorted(pends, jnp.arange(n_blocks) * EXPERT_BLOCK, side='right'), N_EXPERTS - 1)
    h_pad = jnp.concatenate([h, jnp.zeros((1, h.shape[1]), h.dtype)], axis=0)

    def expert_block(args):
        tok, wt, e = args
        return swiglu(h_pad[tok], exp_gate[e], exp_up[e], exp_down[e]) * wt[:, None]

    y = lax.map(expert_block, (slot_tok.reshape(n_blocks, EXPERT_BLOCK),
                               slot_w.reshape(n_blocks, EXPERT_BLOCK), block_e))
    routed = jax.ops.segment_sum(y.reshape(n_slots, -1), slot_tok, num_segments=t + 1)[:t]
    return routed + swiglu(h, sh_gate, sh_up, sh_down)


def setup_inputs(seed: int = 0) -> dict:
    key = jax.random.key(seed)
    ks = jax.random.split(key, 22)

    def nrm(k, shape, scale=1.0):
        return jax.random.normal(k, shape, jnp.float32) * scale

    D = D_MODEL
    L = DEPTH
    return {
        'x': nrm(ks[0], (BATCH, SEQ, D)),
        'c': nrm(ks[1], (BATCH, D)),
        'ctx': nrm(ks[2], (BATCH, CTX_LEN, D)),
        'c_ctx': nrm(ks[3], (D,)),
        'mod_w': nrm(ks[4], (L, D, 6 * D), 0.5 * D ** -0.5),
        'mod_b': nrm(ks[5], (L, 6 * D), 0.02),
        'norm1_w': 1.0 + nrm(ks[6], (L, D), 0.02),
        'w_in': nrm(ks[7], (L, D, IN_COLS), D ** -0.5),
        'q_norm_w': 1.0 + nrm(ks[8], (L, HEAD_DIM), 0.02),
        'k_norm_w': 1.0 + nrm(ks[9], (L, HEAD_DIM), 0.02),
        'w_fourier_out': nrm(ks[10], (L, F_WIDTH, D), F_WIDTH ** -0.5),
        'w_attn_out': nrm(ks[11], (L, ATT_WIDTH, D), ATT_WIDTH ** -0.5),
        'w_out': nrm(ks[12], (L, D, D), D ** -0.5),
        'norm2_w': 1.0 + nrm(ks[13], (L, D), 0.02),
        'router_w': nrm(ks[14], (L, D, N_EXPERTS), D ** -0.5),
        'router_b': nrm(ks[15], (L, N_EXPERTS), 0.01),
        'exp_gate': nrm(ks[16], (L, N_EXPERTS, D, EXPERT_FF), D ** -0.5),
        'exp_up': nrm(ks[17], (L, N_EXPERTS, D, EXPERT_FF), D ** -0.5),
        'exp_down': nrm(ks[18], (L, N_EXPERTS, EXPERT_FF, D), EXPERT_FF ** -0.5),
        'shared_gate': nrm(ks[19], (L, D, SHARED_FF), D ** -0.5),
        'shared_up': nrm(ks[20], (L, D, SHARED_FF), D ** -0.5),
        'shared_down': nrm(ks[21], (L, SHARED_FF, D), SHARED_FF ** -0.5),
    }


def reference(x, c, ctx, c_ctx, mod_w, mod_b, norm1_w, w_in, q_norm_w, k_norm_w,
              w_fourier_out, w_attn_out, w_out, norm2_w, router_w, router_b,
              exp_gate, exp_up, exp_down, shared_gate, shared_up, shared_down):
    b, s, d = x.shape
    n_ctx = ctx.shape[1]
    cos, sin = axial_rope(s)
    cond = jnp.concatenate([c, c_ctx[None]], axis=0)
    cx = ctx
    for l in range(DEPTH):
        last = l == DEPTH - 1
        mod = jax.nn.silu(cond) @ mod_w[l] + mod_b[l]
        sh1, sc1, g1, sh2, sc2, g2 = jnp.split(mod[:b][:, None], 6, axis=-1)
        csh1, csc1, cg1, csh2, csc2, cg2 = jnp.split(mod[b], 6, axis=-1)

        h = modulate(rmsnorm(x, norm1_w[l]), sh1, sc1)
        hc = modulate(rmsnorm(cx, norm1_w[l]), csh1, csc1)
        u, q, k, v, gf, ga = split_projection(h @ w_in[l])
        if last:
            kvc = hc @ w_in[l][:, K_OFF:GF_OFF]
        else:
            pc = hc @ w_in[l]
            kvc = pc[..., K_OFF:GF_OFF]
        kc = rmsnorm(to_heads(kvc[..., :KV_WIDTH], N_KV_HEADS), k_norm_w[l])
        vc = to_heads(kvc[..., KV_WIDTH:], N_KV_HEADS)
        q = apply_rope(rmsnorm(to_heads(q, N_HEADS), q_norm_w[l]), cos, sin)
        k = apply_rope(rmsnorm(to_heads(k, N_KV_HEADS), k_norm_w[l]), cos, sin)
        v = to_heads(v, N_KV_HEADS)
        ya = latent_attention(q, k, v, kc, vc)
        yf = fourier_mix(u)
        x = x + g1 * merge_branches(yf, ya, gf, ga, w_fourier_out[l], w_attn_out[l], w_out[l])

        if not last:
            uc, qc, _, _, gfc, gac = split_projection(pc)
            qc = rmsnorm(to_heads(qc, N_HEADS), q_norm_w[l])
            yac = context_attention(qc, kc, vc)
            yfc = fourier_mix(uc)
            cx = cx + cg1 * merge_branches(yfc, yac, gfc, gac, w_fourier_out[l], w_attn_out[l], w_out[l])

        h2 = modulate(rmsnorm(x, norm2_w[l]), sh2, sc2)
        x = x + g2 * moe(h2.reshape(b * s, d), router_w[l], router_b[l], exp_gate[l], exp_up[l],
                         exp_down[l], shared_gate[l], shared_up[l], shared_down[l]).reshape(b, s, d)
        if not last:
            h2c = modulate(rmsnorm(cx, norm2_w[l]), csh2, csc2)
            cx = cx + cg2 * moe(h2c.reshape(b * n_ctx, d), router_w[l], router_b[l], exp_gate[l], exp_up[l],
                                exp_down[l], shared_gate[l], shared_up[l], shared_down[l]).reshape(b, n_ctx, d)
    return x
```

```python
from contextlib import ExitStack
import numpy as np
import ml_dtypes
import concourse.bass as bass
import concourse.mybir as mybir
from concourse.bass_utils import run_bass_kernel_spmd

F32 = mybir.dt.float32
BF16 = mybir.dt.bfloat16
U8 = mybir.dt.uint8
AF = mybir.ActivationFunctionType
ALU = mybir.AluOpType
AX = mybir.AxisListType

D = 2048
S = 4096
NOWN = 1024
NCTX = 256
NE = 64
EPS = 1e-6
Q_OFF, K_OFF, V_OFF, GF_OFF, GA_OFF = 1024, 3072, 3584, 4096, 6144


class Buf:
    __slots__ = ("name", "lw", "rd", "excl")

    def __init__(self, name="", excl=False):
        self.name = name
        self.lw = None
        self.rd = {}
        self.excl = excl


class Prog:
    ENGS = ("pe", "act", "dve", "pool", "sp")

    def __init__(self, nc):
        self.nc = nc
        self.lists = {e: [] for e in self.ENGS}
        self.cnt = {e: 0 for e in self.ENGS}
        self.waited = {e: {} for e in self.ENGS}
        self.dma_sems = {}
        self.sem_handles = {}
        self._rec = None

    def rec(self, fn, *a, **k):
        self._rec = []
        fn(*a, **k)
        r, self._rec = self._rec, None
        return r

    def play(self, lists):
        idx = [0] * len(lists)
        left = sum(len(l) for l in lists)
        while left:
            for j, l in enumerate(lists):
                if idx[j] < len(l):
                    kind, a, k = l[idx[j]]
                    idx[j] += 1
                    left -= 1
                    getattr(self, kind)(*a, **k)

    def _deps(self, reads, writes):
        deps = []
        for b in reads:
            if b.lw is not None:
                deps.append(b.lw)
        for b in writes:
            if b.lw is not None:
                deps.append(b.lw)
            deps.extend(b.rd.items())
        return deps

    def _emit_waits(self, eng, deps):
        need = {}
        w = self.waited[eng]
        for (k, v) in deps:
            if v > w.get(k, 0) and v > need.get(k, 0):
                need[k] = v
        for k, v in need.items():
            w[k] = v
            self.lists[eng].append(("wait", k, v))

    def _commit(self, tok, reads, writes):
        k, v = tok
        for b in reads:
            if b.rd.get(k, 0) < v:
                b.rd[k] = v
        for b in writes:
            b.lw = tok
            b.rd = {}

    def op(self, eng, fns, reads=(), writes=()):
        if self._rec is not None:
            self._rec.append(("op", (eng, fns, list(reads), list(writes)), {}))
            return None
        if not isinstance(fns, (list, tuple)):
            fns = [fns]
        ex = [b for b in reads if b.excl]
        if ex:
            writes = list(writes) + ex
        self._emit_waits(eng, self._deps(reads, writes))
        self.cnt[eng] += 1
        tok = ("e_" + eng, self.cnt[eng])
        L = self.lists[eng]
        for f in fns[:-1]:
            L.append(("ins", f, None))
        L.append(("ins", fns[-1], ("e_" + eng, 1)))
        self._commit(tok, reads, writes)
        return tok

    def dma(self, eng, fn, reads=(), writes=(), sem=None):
        if self._rec is not None:
            self._rec.append(("dma", (eng, fn, list(reads), list(writes), sem), {}))
            return None
        self._emit_waits(eng, self._deps(reads, writes))
        sem = "d_" + sem
        self.dma_sems[sem] = self.dma_sems.get(sem, 0) + 16
        tok = (sem, self.dma_sems[sem])
        self.lists[eng].append(("ins", fn, (sem, 16)))
        self._commit(tok, reads, writes)
        return tok

    def wait_all(self, eng, bufs):
        deps = [b.lw for b in bufs if b.lw is not None]
        self._emit_waits(eng, deps)

    def emit(self, stack):
        nc = self.nc
        keys = ["e_" + e for e in self.ENGS] + sorted(self.dma_sems.keys())
        for k in keys:
            self.sem_handles[k] = stack.enter_context(nc.semaphore(k))
        block = stack.enter_context(nc.Block())
        H = self.sem_handles

        def run(e, items):
            for it in items:
                if it[0] == "wait":
                    e.wait_ge(H[it[1]], it[2])
                else:
                    ins = it[1](e)
                    if it[2] is not None:
                        ins.then_inc(H[it[2][0]], it[2][1])

        @block.tensor
        def _(e):
            run(e, self.lists["pe"])

        @block.scalar
        def _(e):
            run(e, self.lists["act"])

        @block.vector
        def _(e):
            run(e, self.lists["dve"])

        @block.gpsimd
        def _(e):
            run(e, self.lists["pool"])

        @block.sync
        def _(e):
            run(e, self.lists["sp"])


def I(method, *args, **kw):
    return lambda e: getattr(e, method)(*args, **kw)


def build_program(debug=False, stop_after=99):
    nc = bass.Bass("TRN2", target_bir_lowering=False)

    def din(name, shape, dt=F32):
        return nc.dram_tensor(name, list(shape), dt, kind="ExternalInput").ap()

    xall = din("xall", [S, D])
    ctxb = din("ctxb", [NCTX, D])
    condT = din("condT", [128, 32])
    mod_w = din("mod_w", [D, 6 * D])
    mod_bT = din("mod_bT", [128, 96])
    mod_b = din("mod_b", [1, 6 * D])
    norm1T = din("norm1T", [128, 16])
    norm2T = din("norm2T", [128, 16])
    w_in = din("w_in", [D, 8192])
    qnw = din("qnw", [1, 128])
    knw = din("knw", [1, 128])
    w_fo = din("w_fo", [1024, D])
    w_ao = din("w_ao", [D, D])
    w_o = din("w_o", [D, D])
    router_w = din("router_w", [D, NE])
    router_b = din("router_b", [1, NE])
    exp_gate = din("exp_gate", [NE, D, 512])
    exp_up = din("exp_up", [NE, D, 512])
    exp_down = din("exp_down", [NE, 512, D])
    sh_gate = din("sh_gate", [D, 512])
    sh_up = din("sh_up", [D, 512])
    sh_down = din("sh_down", [512, D])
    identb_d = din("identb", [128, 128], BF16)
    identf_d = din("identf", [128, 128])
    onesb_d = din("onesb", [128, 128], BF16)
    chC_d = din("chC", [128, 128], BF16)
    chS_d = din("chS", [128, 128], BF16)
    cosA = din("cosA", [S, 64])
    sinA = din("sinA", [S, 64])
    dftC = din("dftC", [S, NOWN], BF16)
    dftS = din("dftS", [S, NOWN], BF16)
    yout = nc.dram_tensor("yout", [NOWN, D], F32, kind="ExternalOutput").ap()
    dbg = {}

    st = ExitStack()
    KB = 1024
    arena = st.enter_context(nc.sbuf_tensor("arena", [128, 206 * KB], U8))
    psf = [st.enter_context(nc.psum_tensor("ps%d" % i, [128, 512], F32)) for i in range(8)]
    PSB = [Buf("ps%d" % i, excl=True) for i in range(8)]
    P = Prog(nc)

    occupants = []

    def guard(bufs, guards):
        for b in bufs:
            for g in guards:
                if g.lw is not None and b.rd.get(g.lw[0], 0) < g.lw[1]:
                    b.rd[g.lw[0]] = g.lw[1]
                for k, v in g.rd.items():
                    if b.rd.get(k, 0) < v:
                        b.rd[k] = v

    def carve(off, dims, dt, bufs=None):
        n = int(np.prod(dims))
        sz = 4 if dt == F32 else 2
        end = off + n * sz
        assert end <= 206 * KB, (off, dims)
        ap = arena[:, off:end].bitcast(dt)
        if len(dims) == 2:
            ap = ap.rearrange("p (a b) -> p a b", a=dims[0])
        elif len(dims) == 3:
            ap = ap.rearrange("p (a b c) -> p a b c", a=dims[0], b=dims[1])
        if bufs is not None:
            if isinstance(bufs, Buf):
                bufs = [bufs]
            bufs = list(bufs)
            keep = []
            for (o2, e2, b2) in occupants:
                if o2 < end and off < e2:
                    if b2 not in bufs:
                        guard(bufs, [b2])
                    if not (off <= o2 and e2 <= end):
                        keep.append((o2, e2, b2))
                else:
                    keep.append((o2, e2, b2))
            occupants[:] = keep
            for b in bufs:
                occupants.append((off, end, b))
        return ap

    def ps(i):
        return psf[i][:]

    def psb(i):
        return psf[i][:].bitcast(BF16)

    DBGB = []
    OUTB = []

    def dump(name, ap_sb, shape, dt, bufs):
        if not debug:
            return
        d = nc.dram_tensor(name, list(shape), dt, kind="ExternalOutput").ap()
        dbg[name] = d
        b = Buf("dbg_" + name)
        P.dma("sp", I("dma_start", out=d, in_=ap_sb), reads=bufs, writes=[b], sem="dbg")
        DBGB.append(b)

    def finish():
        P.wait_all("sp", DBGB + OUTB)
        P.emit(st)
        st.close()
        return nc

    def flat(bb):
        return [b for x in bb for b in x]

    Bc = Buf("consts")
    Bmod = Buf("mod")
    Bstat = [Buf("stat%d" % i) for i in range(4)]
    Bwd = Buf("wdense")
    o = [0]

    def cst(dims, dt, buf):
        ap = carve(o[0], dims, dt, buf)
        o[0] += int(np.prod(dims)) * (4 if dt == F32 else 2)
        o[0] = (o[0] + 31) // 32 * 32
        return ap

    identb = cst([128], BF16, Bc)
    identf = cst([128], F32, Bc)
    onesb = cst([128], BF16, Bc)
    chC = cst([128], BF16, Bc)
    chS = cst([128], BF16, Bc)
    qnw_bc = cst([128], F32, Bc)
    knw_bc = cst([128], F32, Bc)
    cond_f = cst([32], F32, Bc)
    modbT = cst([96], F32, Bc)
    n1T = cst([16], F32, Bc)
    n2T = cst([16], F32, Bc)
    rb_bc = cst([NE], F32, Bc)
    scT = cst([16, 2], BF16, Bmod)
    scT_rep = cst([16, 128], BF16, Bmod)
    modA = cst([32, 2], F32, Bmod)
    S1 = cst([16, 2], F32, Bmod)
    modB = cst([32, 2], F32, Bmod)
    S2 = cst([16], F32, Bmod)
    B2 = cst([16], F32, Bmod)
    wdense = cst([8, NE], F32, Bwd)
    stat = [cst([4], F32, Bstat[i]) for i in range(4)]
    assert o[0] <= 12 * KB, o[0]

    YFT_OFF = 12 * KB
    R1 = 28 * KB
    R2 = 60 * KB
    R3 = 128 * KB

    for (dst, src) in ((identb, identb_d[:, :]), (identf, identf_d[:, :]), (onesb, onesb_d[:, :]), (chC, chC_d[:, :]),
                       (chS, chS_d[:, :]), (qnw_bc, qnw[0:1, :].partition_broadcast(128)),
                       (knw_bc, knw[0:1, :].partition_broadcast(128)), (cond_f, condT[:, :]), (modbT, mod_bT[:, :]),
                       (n1T, norm1T[:, :]), (n2T, norm2T[:, :]), (rb_bc, router_b[0:1, :].partition_broadcast(128))):
        P.dma("sp", I("dma_start", out=dst, in_=src), writes=[Bc], sem="c0")

    P.op("act", I("activation", out=scT.rearrange("p a b -> p (a b)"), in_=cond_f, func=AF.Silu), reads=[Bc], writes=[Bmod])
    P.op("dve", I("tensor_copy", out=scT_rep, in_=scT[:, :, 0:1].broadcast_to([128, 16, 128])), reads=[Bmod], writes=[Bmod])

    BMS = [Buf("ms0"), Buf("ms1")]
    mcount = [0]

    def mod_cols(col0, ncols, slot_off, psbank, pscol0, row_form, evac, bw=256, defer=False):
        nblk = ncols // bw
        slots = [carve(slot_off + i * bw * 32, [16, bw], BF16, BMS[i]) for i in range(2)]
        def blk(j):
            s = mcount[0] % 2
            mcount[0] += 1
            c0 = col0 + bw * j
            src = mod_w[:, c0:c0 + bw].rearrange("(c p) f -> p c f", p=128)
            P.dma("pool", I("dma_start", out=slots[s], in_=src), writes=[BMS[s]], sem="ms%d" % s)
            if not row_form:
                for fc in range(bw // 128):
                    ci = (bw // 128) * j + fc
                    outap = ps(psbank)[:, pscol0 + 2 * ci: pscol0 + 2 * ci + 2]
                    fns = [I("matmul", outap, lhsT=slots[s][:, c, fc * 128:(fc + 1) * 128], rhs=scT[:, c, :],
                             start=(c == 0), stop=(c == 15)) for c in range(16)]
                    P.op("pe", fns, reads=[BMS[s], Bmod], writes=[PSB[psbank]])
            else:
                bk = psbank + (j % 2)
                fns = [I("matmul", ps(bk)[:, 0:256], lhsT=scT_rep[:, c, :], rhs=slots[s][:, c, :],
                         start=(c == 0), stop=(c == 15)) for c in range(16)]
                P.op("pe", fns, reads=[BMS[s], Bmod], writes=[PSB[bk]])
                evac(j, bk)

        thunks = [(lambda j: lambda: blk(j))(j) for j in range(nblk)]
        if defer:
            return thunks
        for th in thunks:
            th()

    mod_cols(0, 4096, R3, 7, 0, False, None, bw=512)
    P.op("dve", I("tensor_tensor", out=modA, in0=ps(7)[:, 0:64].rearrange("p (a b) -> p a b", b=2),
                  in1=modbT[:, 0:32].unsqueeze(2).broadcast_to([128, 32, 2]), op=ALU.add), reads=[PSB[7], Bc], writes=[Bmod])
    P.op("dve", I("tensor_scalar", out=S1, in0=modA[:, 16:32, :], scalar1=1.0, scalar2=None, op0=ALU.add), reads=[Bmod], writes=[Bmod])
    P.op("dve", I("tensor_tensor", out=S1, in0=S1, in1=n1T.unsqueeze(2).broadcast_to([128, 16, 2]), op=ALU.mult),
         reads=[Bmod, Bc], writes=[Bmod])

    def nt_load(src_rows, xt, Bx):
        P.dma("sp", I("dma_start", out=xt, in_=src_rows), writes=[Bx], sem=Bx.name)

    def norm_transpose_tile(src_rows, r, xt, xn, si, dst_fn, dst_bufs, Bx, Bxn):
        nt_load(src_rows, xt, Bx)
        nt_compute(r, xt, xn, si, dst_fn, dst_bufs, Bx, Bxn)

    def nt_compute(r, xt, xn, si, dst_fn, dst_bufs, Bx, Bxn, bank0=0):
        stt, Bst = stat[si], Bstat[si]
        P.op("pool", I("memset", stt[:, 0:1], 0.0), writes=[Bst])
        P.op("act", I("activation", out=xn, in_=xt, func=AF.Square, accum_out=stt[:, 0:1]), reads=[Bx], writes=[Bxn, Bst])
        P.op("act", I("activation", out=stt[:, 1:2], in_=stt[:, 0:1], func=AF.Sqrt, scale=1.0 / D, bias=EPS), reads=[Bst], writes=[Bst])
        P.op("dve", I("reciprocal", out=stt[:, 2:3], in_=stt[:, 1:2]), reads=[Bst], writes=[Bst])
        P.op("dve", I("tensor_scalar", out=xn, in0=xt, scalar1=stt[:, 2:3], scalar2=None, op0=ALU.mult), reads=[Bx, Bst], writes=[Bxn])
        for g in range(4):
            bk = bank0 + g % 2
            pv = psb(bk)[:, 0:512]
            fns = [I("transpose", pv[:, j * 128:(j + 1) * 128], xn[:, (4 * g + j) * 128:(4 * g + j + 1) * 128], identb) for j in range(4)]
            P.op("pe", fns, reads=[Bxn, Bc], writes=[PSB[bk]])
            if g % 2 == 0:
                fns = [I("activation", out=dst_fn(4 * g + j), in_=pv[:, j * 128:(j + 1) * 128], func=AF.Identity,
                         scale=S1[:, 4 * g + j, r:r + 1], bias=modA[:, 4 * g + j, r:r + 1]) for j in range(4)]
                P.op("act", fns, reads=[PSB[bk], Bmod], writes=[dst_bufs[g]])
            else:
                fns = [I("tensor_scalar", out=dst_fn(4 * g + j), in0=pv[:, j * 128:(j + 1) * 128], scalar1=S1[:, 4 * g + j, r:r + 1],
                         scalar2=modA[:, 4 * g + j, r:r + 1], op0=ALU.mult, op1=ALU.add) for j in range(4)]
                P.op("dve", fns, reads=[PSB[bk], Bmod], writes=[dst_bufs[g]])

    dump("d_S1", S1.rearrange("p a b -> p (a b)"), [128, 32], F32, [Bmod])
    dump("d_modA", modA.rearrange("p a b -> p (a b)"), [128, 64], F32, [Bmod])
    if stop_after <= 0:
        return finish()

    BWU = [Buf("wu0"), Buf("wu1")]
    WU = carve(R1, [16, 1024], BF16, BWU)
    for h in range(2):
        src = w_in[:, h * 512:(h + 1) * 512].rearrange("(c p) f -> p c f", p=128)
        P.dma("pool", I("dma_start", out=WU[:, :, h * 512:(h + 1) * 512], in_=src), writes=[BWU[h]], sem="wu%d" % h)
    BU = [[Buf("u%d_%d" % (t, n)) for n in range(2)] for t in range(32)]
    U = carve(R2, [32, 1024], BF16, flat(BU))
    BXS = [Buf("xs0"), Buf("xs1")]
    BXN = [Buf("xn0"), Buf("xn1")]
    BHT = [[Buf("ht%d_%d" % (s, g)) for g in range(4)] for s in range(2)]
    XS = [carve(R3 + i * 8 * KB, [D], F32, BXS[i]) for i in range(2)]
    XN = [carve(R3 + 16 * KB + i * 4 * KB, [D], BF16, BXN[i]) for i in range(2)]
    HT = [carve(R3 + 24 * KB + i * 4 * KB, [16, 128], BF16, BHT[i]) for i in range(2)]
    def s1_A(t):
        s = t % 2
        nt_compute(0, XS[s], XN[s], s, (lambda s: lambda c: HT[s][:, c, :])(s), BHT[s], BXS[s], BXN[s])

    def s1_Bmm(t):
        s = t % 2
        for n in range(2):
            bk = 2 + (2 * t + n) % 4
            fns = [I("matmul", ps(bk), lhsT=HT[s][:, c, :], rhs=WU[:, c, n * 512:(n + 1) * 512], start=(c == 0), stop=(c == 15)) for c in range(16)]
            P.op("pe", fns, reads=BHT[s] + [BWU[n]], writes=[PSB[bk]])

    def s1_Bev(t):
        for n in range(2):
            bk = 2 + (2 * t + n) % 4
            if n == 0:
                P.op("act", I("activation", out=U[:, t, n * 512:(n + 1) * 512], in_=ps(bk), func=AF.Identity), reads=[PSB[bk]], writes=[BU[t][n]])
            else:
                P.op("dve", I("tensor_copy", out=U[:, t, n * 512:(n + 1) * 512], in_=ps(bk)), reads=[PSB[bk]], writes=[BU[t][n]])

    nt_load(xall[0:128, :], XS[0], BXS[0])
    for i in range(33):
        if i + 1 < 32:
            nt_load(xall[(i + 1) * 128:(i + 2) * 128, :], XS[(i + 1) % 2], BXS[(i + 1) % 2])
        ch = []
        if i >= 1:
            ch.append(P.rec(s1_Bmm, i - 1))
        if i < 32:
            ch.append(P.rec(s1_A, i))
        P.play(ch)
        if i >= 1:
            s1_Bev(i - 1)
    dump("d_U", U.rearrange("p a b -> p (a b)"), [128, 32 * 1024], BF16, flat(BU))
    if stop_after <= 1:
        return finish()

    BTAB = [Buf("tabc"), Buf("tabs")]
    BABT = [[Buf("abt%d%d" % (i, j)) for j in range(2)] for i in range(2)]
    BYF = [[Buf("yf%d_%d" % (g, kb)) for kb in range(2)] for g in range(8)]
    YFT = carve(YFT_OFF, [8, NOWN], BF16, flat(BYF))
    TABC = carve(R3, [32, 512], BF16, BTAB[0])
    TABS = carve(R3 + 32 * KB, [32, 512], BF16, BTAB[1])
    ABT = [[carve(R3 + 64 * KB + (2 * i + j) * KB, [512], BF16, BABT[i][j]) for j in range(2)] for i in range(2)]
    for kb in range(2):
        for (tab, srcd, Bt, nm) in ((TABC, dftC, BTAB[0], "tabc"), (TABS, dftS, BTAB[1], "tabs")):
            for hf in range(2):
                src = srcd[hf * 2048:(hf + 1) * 2048, kb * 512:(kb + 1) * 512].rearrange("(t p) k -> p t k", p=128)
                P.dma("sp", I("dma_start", out=tab[:, hf * 16:(hf + 1) * 16, :], in_=src), writes=[Bt], sem=nm)
        for g in range(8):
            i = g % 2
            ba, bb_, by = 0 + i, 2 + i, 4 + i
            ub = [BU[t][g // 4] for t in range(32)]
            fns = [I("matmul", ps(ba), lhsT=U[:, t, g * 128:(g + 1) * 128], rhs=TABC[:, t, :], start=(t == 0), stop=(t == 31)) for t in range(32)]
            P.op("pe", fns, reads=ub + [BTAB[0]], writes=[PSB[ba]])
            fns = [I("matmul", ps(bb_), lhsT=U[:, t, g * 128:(g + 1) * 128], rhs=TABS[:, t, :], start=(t == 0), stop=(t == 31)) for t in range(32)]
            P.op("pe", fns, reads=ub + [BTAB[1]], writes=[PSB[bb_]])
            P.op("act", I("activation", out=ABT[i][0], in_=ps(ba), func=AF.Identity), reads=[PSB[ba]], writes=[BABT[i][0]])
            P.op("dve", I("tensor_copy", out=ABT[i][1], in_=ps(bb_)), reads=[PSB[bb_]], writes=[BABT[i][1]])
            fns = [I("matmul", ps(by), lhsT=chC, rhs=ABT[i][0], start=True, stop=False),
                   I("matmul", ps(by), lhsT=chS, rhs=ABT[i][1], start=False, stop=True)]
            P.op("pe", fns, reads=[BABT[i][0], BABT[i][1], Bc], writes=[PSB[by]])
            if i == 0:
                P.op("act", I("activation", out=YFT[:, g, kb * 512:(kb + 1) * 512], in_=ps(by), func=AF.Identity), reads=[PSB[by]], writes=[BYF[g][kb]])
            else:
                P.op("dve", I("tensor_copy", out=YFT[:, g, kb * 512:(kb + 1) * 512], in_=ps(by)), reads=[PSB[by]], writes=[BYF[g][kb]])
    dump("d_YFT", YFT.rearrange("p a b -> p (a b)"), [128, 8 * 1024], BF16, flat(BYF))
    if stop_after <= 2:
        return finish()

    BHTO = [[Buf("hto%d_%d" % (t, g)) for g in range(4)] for t in range(8)]
    HTO = carve(R1, [16, NOWN], BF16, flat(BHTO))
    BKT = [Buf("kt%d" % t) for t in range(34)]
    BVS = [Buf("vs%d" % t) for t in range(34)]
    KT = carve(R2, [4, 4352], BF16, BKT)
    VS = carve(R2 + 34 * KB, [34, 512], BF16, BVS)
    BWKV = [Buf("wkv0"), Buf("wkv1")]
    WKV = carve(R3, [16, 1024], BF16, BWKV)
    for h in range(2):
        src = w_in[:, K_OFF + h * 512:K_OFF + (h + 1) * 512].rearrange("(c p) f -> p c f", p=128)
        P.dma("pool", I("dma_start", out=WKV[:, :, h * 512:(h + 1) * 512], in_=src), writes=[BWKV[h]], sem="wkv%d" % h)
    o3 = R3 + 32 * KB
    BXS = [Buf("xs0b"), Buf("xs1b")]
    BXN = [Buf("xn0b"), Buf("xn1b")]
    BHT = [[Buf("htb%d_%d" % (s, g)) for g in range(4)] for s in range(2)]
    XS = [carve(o3 + i * 8 * KB, [D], F32, BXS[i]) for i in range(2)]
    XN = [carve(o3 + 16 * KB + i * 4 * KB, [D], BF16, BXN[i]) for i in range(2)]
    HT = [carve(o3 + 24 * KB + i * 4 * KB, [16, 128], BF16, BHT[i]) for i in range(2)]
    o3 += 32 * KB
    BSQ, BKN, BT12, BT34, BKR, BSSK = Buf("sq"), Buf("kn"), Buf("t12"), Buf("t34"), Buf("kr"), Buf("ssk")
    BCS = [Buf("cs0"), Buf("cs1")]
    SQ = carve(o3, [512], F32, BSQ)
    KN = carve(o3 + 2 * KB, [512], F32, BKN)
    T1 = carve(o3 + 4 * KB, [256], F32, BT12)
    T2 = carve(o3 + 5 * KB, [256], F32, BT12)
    T3 = carve(o3 + 6 * KB, [256], F32, BT34)
    T4 = carve(o3 + 7 * KB, [256], F32, BT34)
    KR = carve(o3 + 8 * KB, [512], BF16, BKR)
    CS = [carve(o3 + 9 * KB + i * 512, [128], F32, BCS[i]) for i in range(2)]
    SSK = carve(o3 + 10 * KB, [16], F32, BSSK)

    def qk_norm_rope(psbank, nw_bc, rope_slot, out_bf):
        Bp = PSB[psbank]
        h4 = lambda ap: ap.rearrange("p (h d) -> p h d", h=4)
        P.op("act", I("activation", out=SQ, in_=ps(psbank), func=AF.Square), reads=[Bp], writes=[BSQ])
        P.op("dve", I("tensor_reduce", out=SSK[:, 0:4], in_=h4(SQ), axis=AX.X, op=ALU.add), reads=[BSQ], writes=[BSSK])
        P.op("act", I("activation", out=SSK[:, 4:8], in_=SSK[:, 0:4], func=AF.Sqrt, scale=1.0 / 128, bias=EPS), reads=[BSSK], writes=[BSSK])
        P.op("dve", I("reciprocal", out=SSK[:, 8:12], in_=SSK[:, 4:8]), reads=[BSSK], writes=[BSSK])
        P.op("dve", I("tensor_tensor", out=h4(KN), in0=h4(ps(psbank)), in1=nw_bc.unsqueeze(1).broadcast_to([128, 4, 128]), op=ALU.mult),
             reads=[Bp, Bc], writes=[BKN])
        rbc = SSK[:, 8:12].unsqueeze(2).broadcast_to([128, 4, 128])
        if rope_slot is None:
            P.op("dve", I("tensor_tensor", out=h4(out_bf), in0=h4(KN), in1=rbc, op=ALU.mult), reads=[BKN, BSSK], writes=[BKR])
            return
        P.op("dve", I("tensor_tensor", out=h4(KN), in0=h4(KN), in1=rbc, op=ALU.mult), reads=[BKN, BSSK], writes=[BKN])
        kn5 = KN.rearrange("p (h x y f) -> p h x y f", h=4, x=2, y=2)
        xa, xb = kn5[:, :, :, 0, :], kn5[:, :, :, 1, :]
        o5 = out_bf.rearrange("p (h x y f) -> p h x y f", h=4, x=2, y=2)
        oa, ob = o5[:, :, :, 0, :], o5[:, :, :, 1, :]
        cs = CS[rope_slot]
        cb = cs[:, 0:64].rearrange("p (x f) -> p x f", x=2).unsqueeze(1).broadcast_to([128, 4, 2, 32])
        sb = cs[:, 64:128].rearrange("p (x f) -> p x f", x=2).unsqueeze(1).broadcast_to([128, 4, 2, 32])
        v4 = lambda ap: ap.rearrange("p (h x f) -> p h x f", h=4, x=2)
        Bcs = BCS[rope_slot]
        P.op("dve", [I("tensor_tensor", out=v4(T1), in0=xa, in1=cb, op=ALU.mult), I("tensor_tensor", out=v4(T2), in0=xb, in1=sb, op=ALU.mult)],
             reads=[BKN, Bcs], writes=[BT12])
        P.op("pool", [I("tensor_tensor", out=v4(T3), in0=xb, in1=cb, op=ALU.mult), I("tensor_tensor", out=v4(T4), in0=xa, in1=sb, op=ALU.mult)],
             reads=[BKN, Bcs], writes=[BT34])
        P.op("dve", I("tensor_tensor", out=oa, in0=v4(T1), in1=v4(T2), op=ALU.subtract), reads=[BT12], writes=[BKR])
        P.op("pool", I("tensor_tensor", out=ob, in0=v4(T3), in1=v4(T4), op=ALU.add), reads=[BT34, BKR], writes=[BKR])

    def load_cs(slot, t):
        P.dma("sp", I("dma_start", out=CS[slot][:, 0:64], in_=cosA[t * 128:(t + 1) * 128, :]), writes=[BCS[slot]], sem="cs%d" % slot)
        P.dma("sp", I("dma_start", out=CS[slot][:, 64:128], in_=sinA[t * 128:(t + 1) * 128, :]), writes=[BCS[slot]], sem="cs%d" % slot)

    def s3_tile(t):
        s = t % 2
        if t < 8:
            return (lambda t: lambda c: HTO[:, c, t * 128:(t + 1) * 128])(t), BHTO[t]
        return (lambda s: lambda c: HT[s][:, c, :])(s), BHT[s]

    def s3_rows(t):
        return xall[t * 128:(t + 1) * 128, :] if t < 32 else ctxb[(t - 32) * 128:(t - 31) * 128, :]

    def s3_A(t):
        s = t % 2
        dst_fn, dbufs = s3_tile(t)
        nt_compute(0 if t < 32 else 1, XS[s], XN[s], s, dst_fn, dbufs, BXS[s], BXN[s])

    def s3_B(t):
        s = t % 2
        dst_fn, dbufs = s3_tile(t)
        bkk, bkv = 2 + s, 4 + s
        fns = [I("matmul", ps(bkk), lhsT=dst_fn(c), rhs=WKV[:, c, 0:512], start=(c == 0), stop=(c == 15)) for c in range(16)]
        P.op("pe", fns, reads=dbufs + [BWKV[0]], writes=[PSB[bkk]])
        fns = [I("matmul", ps(bkv), lhsT=dst_fn(c), rhs=WKV[:, c, 512:1024], start=(c == 0), stop=(c == 15)) for c in range(16)]
        P.op("pe", fns, reads=dbufs + [BWKV[1]], writes=[PSB[bkv]])

    def s3_Bev(t):
        bkv = 4 + t % 2
        P.op("act", I("activation", out=VS[:, t, :], in_=ps(bkv), func=AF.Identity), reads=[PSB[bkv]], writes=[BVS[t]])

    def s3_C(t):
        s = t % 2
        qk_norm_rope(2 + s, knw_bc, s if t < 32 else None, KR)
        bt = 6 + s
        pv = psb(bt)[:, 0:512]
        fns = [I("transpose", pv[:, h * 128:(h + 1) * 128], KR[:, h * 128:(h + 1) * 128], identb) for h in range(4)]
        P.op("pe", fns, reads=[BKR, Bc], writes=[PSB[bt]])
        P.op("dve", I("tensor_copy", out=KT[:, :, t * 128:(t + 1) * 128], in_=pv.rearrange("p (h d) -> p h d", h=4)), reads=[PSB[bt]], writes=[BKT[t]])

    nt_load(s3_rows(0), XS[0], BXS[0])
    load_cs(0, 0)
    load_cs(1, 1)
    for i in range(36):
        if i + 1 < 34:
            nt_load(s3_rows(i + 1), XS[(i + 1) % 2], BXS[(i + 1) % 2])
        ch = []
        if 0 <= i - 1 < 34:
            ch.append(P.rec(s3_B, i - 1))
        if i < 34:
            ch.append(P.rec(s3_A, i))
        if 0 <= i - 2 < 34:
            ch.append(P.rec(s3_C, i - 2))
        P.play(ch)
        if 0 <= i - 1 < 34:
            s3_Bev(i - 1)
        if 0 <= i - 2 < 34 and i < 32:
            load_cs(i % 2, i)
    dump("d_KT", KT.rearrange("p a b -> p (a b)"), [128, 4 * 4352], BF16, BKT)
    dump("d_VS", VS.rearrange("p a b -> p (a b)"), [128, 34 * 512], BF16, BVS)
    dump("d_HTO", HTO.rearrange("p a b -> p (a b)"), [128, 16 * 1024], BF16, flat(BHTO))
    if stop_after <= 3:
        return finish()

    BQT = [[Buf("qt%d_%d" % (qb, t)) for t in range(8)] for qb in range(4)]
    QT = carve(R3, [16, NOWN], BF16, flat(BQT))
    BWQ = [Buf("wq0"), Buf("wq1")]
    WQ = [carve(R3 + 32 * KB + i * 16 * KB, [16, 512], BF16, BWQ[i]) for i in range(2)]
    def wq_load(qblk):
        src = w_in[:, Q_OFF + qblk * 512:Q_OFF + (qblk + 1) * 512].rearrange("(c p) f -> p c f", p=128)
        P.dma("pool", I("dma_start", out=WQ[qblk % 2], in_=src), writes=[BWQ[qblk % 2]], sem="wq%d" % (qblk % 2))

    wq_load(0)
    for qblk in range(4):
        s = qblk % 2
        if qblk + 1 < 4:
            wq_load(qblk + 1)
        for t in range(8):
            cs_s = t % 2
            load_cs(cs_s, t)
            bq = 2 + (t % 2)
            fns = [I("matmul", ps(bq), lhsT=HTO[:, c, t * 128:(t + 1) * 128], rhs=WQ[s][:, c, :], start=(c == 0), stop=(c == 15)) for c in range(16)]
            P.op("pe", fns, reads=BHTO[t] + [BWQ[s]], writes=[PSB[bq]])
            qk_norm_rope(bq, qnw_bc, cs_s, KR)
            bt = 6 + (t % 2)
            pv = psb(bt)[:, 0:512]
            fns = [I("transpose", pv[:, h * 128:(h + 1) * 128], KR[:, h * 128:(h + 1) * 128], identb) for h in range(4)]
            P.op("pe", fns, reads=[BKR, Bc], writes=[PSB[bt]])
            P.op("dve", I("tensor_copy", out=QT[:, 4 * qblk:4 * qblk + 4, t * 128:(t + 1) * 128], in_=pv.rearrange("p (h d) -> p h d", h=4)),
                 reads=[PSB[bt]], writes=[BQT[qblk][t]])
    dump("d_QT", QT.rearrange("p a b -> p (a b)"), [128, 16 * 1024], BF16, flat(BQT))
    if stop_after <= 4:
        return finish()

    BYA = [[Buf("ya%d_%d" % (h, qb)) for qb in range(2)] for h in range(16)]
    YAT = carve(R1, [16, NOWN], BF16, flat(BYA))
    o5_ = R3 + 32 * KB
    BPT = [Buf("pt%d" % i) for i in range(4)]
    BRD = [Buf("rd0"), Buf("rd1")]
    PT = [carve(o5_ + i * KB, [512], BF16, BPT[i]) for i in range(4)]
    RD = [carve(o5_ + 4 * KB + i * 2 * KB, [512], F32, BRD[i]) for i in range(2)]
    SCALE = 128.0 ** -0.5
    it = 0
    pend_a = mod_cols(3 * D, 2 * D, R3 + 40 * KB, 7, 64, False, None, defer=True)

    def mod2_finish():
        P.op("dve", I("tensor_tensor", out=modB, in0=ps(7)[:, 64:128].rearrange("p (a b) -> p a b", b=2),
                      in1=modbT[:, 48:80].unsqueeze(2).broadcast_to([128, 32, 2]), op=ALU.add), reads=[PSB[7], Bc], writes=[Bmod])
        P.op("dve", I("tensor_scalar", out=S2, in0=modB[:, 16:32, 0], scalar1=1.0, scalar2=None, op0=ALU.add), reads=[Bmod], writes=[Bmod])
        P.op("dve", I("tensor_tensor", out=S2, in0=S2, in1=n2T, op=ALU.mult), reads=[Bmod, Bc], writes=[Bmod])
        P.op("dve", I("tensor_copy", out=B2, in_=modB[:, 0:16, 0]), reads=[Bmod], writes=[Bmod])

    for h in range(16):
        kvh = h // 4
        for qb in range(2):
            par = it % 2
            it += 1
            if pend_a:
                pend_a.pop(0)()
                if not pend_a:
                    mod2_finish()
            bo, bd = 3 + par, 5 + par
            qbufs = [BQT[h // 4][4 * qb + j] for j in range(4)]
            qrhs = QT[:, h, qb * 512:(qb + 1) * 512]

            def emit_s(kt):
                bs = kt % 3
                P.op("pe", I("matmul", ps(bs), lhsT=KT[:, kvh, kt * 128:(kt + 1) * 128], rhs=qrhs, start=True, stop=True),
                     reads=[BKT[kt]] + qbufs, writes=[PSB[bs]])

            emit_s(0)
            emit_s(1)
            for kt in range(34):
                if kt + 2 < 34:
                    emit_s(kt + 2)
                bs = kt % 3
                pi = kt % 4
                P.op("act", I("activation", out=PT[pi], in_=ps(bs), func=AF.Exp, scale=SCALE), reads=[PSB[bs]], writes=[BPT[pi]])
                fns = [I("matmul", ps(bo), lhsT=VS[:, kt, kvh * 128:(kvh + 1) * 128], rhs=PT[pi], start=(kt == 0), stop=(kt == 33)),
                       I("matmul", ps(bd), lhsT=onesb, rhs=PT[pi], start=(kt == 0), stop=(kt == 33))]
                P.op("pe", fns, reads=[BVS[kt], BPT[pi], Bc], writes=[PSB[bo], PSB[bd]])
            P.op("dve", I("reciprocal", out=RD[par], in_=ps(bd)), reads=[PSB[bd]], writes=[BRD[par]])
            P.op("dve", I("tensor_tensor", out=YAT[:, h, qb * 512:(qb + 1) * 512], in0=ps(bo), in1=RD[par], op=ALU.mult),
                 reads=[PSB[bo], BRD[par]], writes=[BYA[h][qb]])
    dump("d_YAT", YAT.rearrange("p a b -> p (a b)"), [128, 16 * 1024], BF16, flat(BYA))
    if stop_after <= 5:
        return finish()

    BHTO = [[Buf("hto2_%d_%d" % (t, g)) for g in range(4)] for t in range(8)]
    HTO = carve(R2, [16, NOWN], BF16, flat(BHTO))
    BYT = [[Buf("yt%d_%d" % (dc, tb)) for tb in range(2)] for dc in range(16)]
    YT = carve(R2 + 32 * KB, [16, NOWN], BF16, flat(BYT))
    o6 = R3 + 56 * KB
    BXS6, BXN6 = [Buf("xs6"), Buf("xs6b")], [Buf("xn6"), Buf("xn6b")]
    XS6 = [carve(o6, [D], F32, BXS6[0]), carve(R3 + 28 * KB, [D], F32, BXS6[1])]
    XN6 = [carve(o6 + 8 * KB, [D], BF16, BXN6[0]), carve(R3 + 36 * KB, [D], BF16, BXN6[1])]

    def s6_A(t):
        i = t % 2
        nt_compute(0, XS6[i], XN6[i], i, (lambda t: lambda c: HTO[:, c, t * 128:(t + 1) * 128])(t), BHTO[t], BXS6[i], BXN6[i], bank0=2 * i)

    for t in range(0, 8, 2):
        for i in range(2):
            nt_load(xall[(t + i) * 128:(t + i + 1) * 128, :], XS6[i], BXS6[i])
        P.play([P.rec(s6_A, t), P.rec(s6_A, t + 1)])
    BWS = [Buf("ws0"), Buf("ws1")]
    WS = [carve(R3 + i * 28 * KB, [56, 256], BF16, BWS[i]) for i in range(2)]
    BSG = [Buf("sg%d" % i) for i in range(4)]
    SG = [carve(o6 + 12 * KB + i * 2 * KB, [512], F32, BSG[i]) for i in range(4)]
    step = 0
    def ws_load(cb):
        s = cb % 2
        c0 = cb * 256
        srcs = [(w_fo[:, c0:c0 + 256], 0, 8), (w_ao[:, c0:c0 + 256], 8, 16),
                (w_in[:, GF_OFF + c0:GF_OFF + c0 + 256], 24, 16), (w_in[:, GA_OFF + c0:GA_OFF + c0 + 256], 40, 16)]
        for (srcd, k0, nk) in srcs:
            src = srcd.rearrange("(c p) f -> p c f", p=128)
            P.dma("pool", I("dma_start", out=WS[s][:, k0:k0 + nk, :], in_=src), writes=[BWS[s]], sem="ws%d" % s)

    ws_load(0)
    for cb in range(8):
        s = cb % 2
        if cb + 1 < 8:
            ws_load(cb + 1)
        for cc in range(2):
            dc = 2 * cb + cc
            for tb in range(2):
                par = step % 2
                step += 1
                b_fo, b_ao, b_gf, b_ga = 4 * par, 4 * par + 1, 4 * par + 2, 4 * par + 3
                tsl = slice(tb * 512, (tb + 1) * 512)
                csl = slice(cc * 128, (cc + 1) * 128)
                fns = [I("matmul", ps(b_fo), lhsT=WS[s][:, k, csl], rhs=YFT[:, k, tsl], start=(k == 0), stop=(k == 7)) for k in range(8)]
                P.op("pe", fns, reads=[BWS[s]] + [BYF[g][tb] for g in range(8)], writes=[PSB[b_fo]])
                fns = [I("matmul", ps(b_ao), lhsT=WS[s][:, 8 + k, csl], rhs=YAT[:, k, tsl], start=(k == 0), stop=(k == 15)) for k in range(16)]
                P.op("pe", fns, reads=[BWS[s]] + [BYA[h][tb] for h in range(16)], writes=[PSB[b_ao]])
                hbufs = [b for t in range(4 * tb, 4 * tb + 4) for b in BHTO[t]]
                fns = [I("matmul", ps(b_gf), lhsT=WS[s][:, 24 + k, csl], rhs=HTO[:, k, tsl], start=(k == 0), stop=(k == 15)) for k in range(16)]
                P.op("pe", fns, reads=[BWS[s]] + hbufs, writes=[PSB[b_gf]])
                fns = [I("matmul", ps(b_ga), lhsT=WS[s][:, 40 + k, csl], rhs=HTO[:, k, tsl], start=(k == 0), stop=(k == 15)) for k in range(16)]
                P.op("pe", fns, reads=[BWS[s]] + hbufs, writes=[PSB[b_ga]])
                P.op("act", I("activation", out=SG[0], in_=ps(b_gf), func=AF.Sigmoid), reads=[PSB[b_gf]], writes=[BSG[0]])
                P.op("act", I("activation", out=SG[1], in_=ps(b_ga), func=AF.Sigmoid), reads=[PSB[b_ga]], writes=[BSG[1]])
                P.op("dve", I("tensor_tensor", out=SG[2], in0=ps(b_fo), in1=SG[0], op=ALU.mult), reads=[PSB[b_fo], BSG[0]], writes=[BSG[2]])
                P.op("dve", I("tensor_tensor", out=SG[3], in0=ps(b_ao), in1=SG[1], op=ALU.mult), reads=[PSB[b_ao], BSG[1]], writes=[BSG[3]])
                P.op("pool", I("tensor_tensor", out=YT[:, dc, tsl], in0=SG[2], in1=SG[3], op=ALU.add), reads=[BSG[2], BSG[3]], writes=[BYT[dc][tb]])
    dump("d_YT", YT.rearrange("p a b -> p (a b)"), [128, 16 * 1024], BF16, flat(BYT))
    if stop_after <= 6:
        return finish()

    BG1 = Buf("g1")
    G1 = carve(12 * KB, [D], F32, BG1)
    P.dma("sp", I("dma_start", out=G1, in_=mod_b[0:1, 2 * D:3 * D].partition_broadcast(128)), writes=[BG1], sem="g1")

    def evac_g1(j, bk):
        P.op("dve", I("tensor_tensor", out=G1[:, j * 256:(j + 1) * 256], in0=ps(bk)[:, 0:256], in1=G1[:, j * 256:(j + 1) * 256], op=ALU.add),
             reads=[PSB[bk]], writes=[BG1])

    mod_cols(2 * D, D, R1, 0, 0, True, evac_g1)
    dump("d_G1", G1, [128, D], F32, [BG1])

    BX1 = [[Buf("x1_%d_%d" % (t, db)) for db in range(4)] for t in range(8)]
    X1 = carve(R3, [8, D], F32, flat(BX1))
    for t in range(8):
        P.dma("sp", I("dma_start", out=X1[:, t, :], in_=xall[t * 128:(t + 1) * 128, :]), writes=BX1[t], sem="x1_%d" % t)
    BWO = [Buf("wo0"), Buf("wo1")]
    WO = [carve(R1 + 16 * KB + i * 16 * KB, [16, 512], BF16, BWO[i]) for i in range(2)]
    BTMP = [Buf("tmp0"), Buf("tmp1")]
    TMP = [carve(R2 + 16 * KB + i * 2 * KB, [512], F32, BTMP[i]) for i in range(2)]
    def wo_load(db):
        src = w_o[:, db * 512:(db + 1) * 512].rearrange("(c p) f -> p c f", p=128)
        P.dma("pool", I("dma_start", out=WO[db % 2], in_=src), writes=[BWO[db % 2]], sem="wo%d" % (db % 2))

    BG2F, BG2 = Buf("g2f"), Buf("g2")
    G2F = carve(36 * KB, [D], F32, BG2F)
    G2 = carve(124 * KB, [D], BF16, BG2)
    P.dma("sp", I("dma_start", out=G2F, in_=mod_b[0:1, 5 * D:6 * D].partition_broadcast(128)), writes=[BG2F], sem="g2f")

    def evac_g2(j, bk):
        P.op("dve", I("tensor_tensor", out=G2[:, j * 256:(j + 1) * 256], in0=ps(bk)[:, 0:256], in1=G2F[:, j * 256:(j + 1) * 256], op=ALU.add),
             reads=[PSB[bk], BG2F], writes=[BG2])

    pending = mod_cols(5 * D, D, 20 * KB, 0, 0, True, evac_g2, defer=True)
    wo_load(0)
    for db in range(4):
        s = db % 2
        if db + 1 < 4:
            wo_load(db + 1)
        for t in range(8):
            if pending:
                pending.pop(0)()
            bk = 2 + (t % 4)
            i = t % 2
            fns = [I("matmul", ps(bk), lhsT=YT[:, k, t * 128:(t + 1) * 128], rhs=WO[s][:, k, :], start=(k == 0), stop=(k == 15)) for k in range(16)]
            P.op("pe", fns, reads=[BYT[k][t // 4] for k in range(16)] + [BWO[s]], writes=[PSB[bk]])
            P.op("dve", I("tensor_tensor", out=TMP[i], in0=ps(bk), in1=G1[:, db * 512:(db + 1) * 512], op=ALU.mult),
                 reads=[PSB[bk], BG1], writes=[BTMP[i]])
            xv = X1[:, t, db * 512:(db + 1) * 512]
            P.op("dve", I("tensor_tensor", out=xv, in0=xv, in1=TMP[i], op=ALU.add), reads=[BTMP[i]], writes=[BX1[t][db]])
    if debug:
        d = nc.dram_tensor("d_X1", [NOWN, D], F32, kind="ExternalOutput").ap()
        b = Buf("dbg_x1")
        for t in range(8):
            P.dma("sp", I("dma_start", out=d[t * 128:(t + 1) * 128, :], in_=X1[:, t, :]), reads=BX1[t], writes=[b], sem="dbg")
        DBGB.append(b)
    if stop_after <= 7:
        return finish()

    while pending:
        pending.pop(0)()
    dump("d_G2", G2, [128, D], BF16, [BG2])
    dump("d_S2", S2, [128, 16], F32, [Bmod])
    if stop_after <= 7.5:
        return finish()

    BH2 = [[Buf("h2_%d_%d" % (t, g)) for g in range(4)] for t in range(8)]
    H2T = carve(R1, [16, NOWN], BF16, flat(BH2))
    o8 = R2
    BRW, BJ, BSC, BRT, BSM = Buf("rw"), Buf("junk"), Buf("sc"), Buf("rt"), Buf("sm")
    BXN2 = [Buf("xn2_0"), Buf("xn2_1")]
    BH2F = [[Buf("h2f%d_%d" % (i, g)) for g in range(4)] for i in range(2)]
    RW = carve(o8, [16, NE], F32, BRW)
    XN2 = [carve(o8 + 4 * KB + i * 8 * KB, [D], F32, BXN2[i]) for i in range(2)]
    H2F = [carve(o8 + 20 * KB + i * 8 * KB, [16, 128], F32, BH2F[i]) for i in range(2)]
    JUNK = carve(o8 + 36 * KB, [D], BF16, BJ)
    SC = carve(o8 + 40 * KB, [8, NE], F32, BSC)
    RT = [carve(o8 + 42 * KB + i * 2 * KB, [8, NE], F32, BRT) for i in range(4)]
    SM = carve(o8 + 50 * KB, [256], F32, BSM)
    P.dma("sp", I("dma_start", out=RW, in_=router_w.rearrange("(c p) f -> p c f", p=128)), writes=[BRW], sem="rw")

    def s8_A(t):
        s = t % 4
        stt = stat[s]
        xn2 = XN2[t % 2]
        P.op("pool", I("memset", stt[:, 0:1], 0.0), writes=[Bstat[s]])
        P.op("act", I("activation", out=JUNK, in_=X1[:, t, :], func=AF.Square, accum_out=stt[:, 0:1]), reads=BX1[t], writes=[BJ, Bstat[s]])
        P.op("act", I("activation", out=stt[:, 1:2], in_=stt[:, 0:1], func=AF.Sqrt, scale=1.0 / D, bias=EPS), reads=[Bstat[s]], writes=[Bstat[s]])
        P.op("dve", I("reciprocal", out=stt[:, 2:3], in_=stt[:, 1:2]), reads=[Bstat[s]], writes=[Bstat[s]])
        P.op("dve", I("tensor_scalar", out=xn2, in0=X1[:, t, :], scalar1=stt[:, 2:3], scalar2=None, op0=ALU.mult), reads=BX1[t] + [Bstat[s]], writes=[BXN2[t % 2]])

    def s8_B(t):
        xn2, h2f = XN2[t % 2], H2F[t % 2]
        for g in range(4):
            bk = g
            fns = [I("transpose", ps(bk)[:, j * 128:(j + 1) * 128], xn2[:, (4 * g + j) * 128:(4 * g + j + 1) * 128], identf) for j in range(4)]
            P.op("pe", fns, reads=[BXN2[t % 2], Bc], writes=[PSB[bk]])
            fns = [I("activation", out=H2T[:, 4 * g + j, t * 128:(t + 1) * 128], in_=ps(bk)[:, j * 128:(j + 1) * 128], func=AF.Identity,
                     scale=S2[:, 4 * g + j:4 * g + j + 1], bias=B2[:, 4 * g + j:4 * g + j + 1]) for j in range(4)]
            P.op("act", fns, reads=[PSB[bk], Bmod], writes=[BH2[t][g]])
            fns = [I("tensor_scalar", out=h2f[:, 4 * g + j, :], in0=ps(bk)[:, j * 128:(j + 1) * 128], scalar1=S2[:, 4 * g + j:4 * g + j + 1],
                     scalar2=B2[:, 4 * g + j:4 * g + j + 1], op0=ALU.mult, op1=ALU.add) for j in range(4)]
            P.op("dve", fns, reads=[PSB[bk], Bmod], writes=[BH2F[t % 2][g]])

    def s8_C(t):
        h2f = H2F[t % 2]
        br = 4 + (t % 2)
        fns = [I("matmul", ps(br)[:, 0:NE], lhsT=h2f[:, c, :], rhs=RW[:, c, :], start=(c == 0), stop=(c == 15)) for c in range(16)]
        P.op("pe", fns, reads=BH2F[t % 2] + [BRW], writes=[PSB[br]])

    def s8_Cev(t):
        br = 4 + (t % 2)
        P.op("act", I("activation", out=SC[:, t, :], in_=ps(br)[:, 0:NE], func=AF.Sigmoid), reads=[PSB[br]], writes=[BSC])

    for i in range(10):
        ch = []
        if i < 8:
            ch.append(P.rec(s8_A, i))
        if 0 <= i - 1 < 8:
            ch.append(P.rec(s8_B, i - 1))
        if 0 <= i - 2 < 8:
            ch.insert(0, P.rec(s8_C, i - 2))
        P.play(ch)
        if 0 <= i - 2 < 8:
            s8_Cev(i - 2)
    if stop_after <= 7.7:
        dump("d_H2T", H2T.rearrange("p a b -> p (a b)"), [128, 16 * 1024], BF16, flat(BH2))
        dump("d_SC", SC.rearrange("p a b -> p (a b)"), [128, 8 * NE], F32, [BSC])
        return finish()
    BI, M1, GS, PEN = RT[0], SM[:, 0:64], SM[:, 64:128], SM[:, 128:192]
    TOP = SM[:, 192:200]
    THR = SM[:, 200:216]
    WS_ = SM[:, 216:232]
    g8 = lambda ap: ap.rearrange("p t (g k) -> p (t g) k", k=8)
    bi4, e4 = g8(BI), g8(RT[1])
    P.op("dve", I("tensor_tensor", out=BI, in0=SC, in1=rb_bc.unsqueeze(1).broadcast_to([128, 8, NE]), op=ALU.add), reads=[BSC, Bc], writes=[BRT])
    P.op("dve", I("tensor_reduce", out=M1, in_=bi4, axis=AX.X, op=ALU.max), reads=[BRT], writes=[BSM])
    P.op("dve", I("tensor_tensor", out=e4, in0=bi4, in1=M1.unsqueeze(2).broadcast_to([128, 64, 8]), op=ALU.is_equal), reads=[BRT, BSM], writes=[BRT])
    P.op("dve", I("scalar_tensor_tensor", out=e4, in0=e4, scalar=-1e30, in1=bi4, op0=ALU.mult, op1=ALU.add), reads=[BRT], writes=[BRT])
    P.op("dve", I("tensor_reduce", out=GS, in_=e4, axis=AX.X, op=ALU.max), reads=[BRT], writes=[BSM])
    P.op("dve", I("tensor_tensor", out=GS, in0=GS, in1=M1, op=ALU.add), reads=[BSM], writes=[BSM])
    for t in range(8):
        P.op("dve", I("max", out=TOP, in_=GS[:, t * 8:(t + 1) * 8]), reads=[BSM], writes=[BSM])
        P.op("dve", I("tensor_copy", out=THR[:, t:t + 1], in_=TOP[:, 3:4]), reads=[BSM], writes=[BSM])
    gs3 = GS.rearrange("p (t g) -> p t g", g=8)
    pen3 = PEN.rearrange("p (t g) -> p t g", g=8)
    P.op("dve", I("tensor_tensor", out=pen3, in0=gs3, in1=THR[:, 0:8].unsqueeze(2).broadcast_to([128, 8, 8]), op=ALU.is_ge), reads=[BSM], writes=[BSM])
    P.op("dve", I("tensor_scalar", out=PEN, in0=PEN, scalar1=1e30, scalar2=-1e30, op0=ALU.mult, op1=ALU.add), reads=[BSM], writes=[BSM])
    MK = RT[2]
    P.op("dve", I("tensor_tensor", out=g8(MK), in0=bi4, in1=PEN.unsqueeze(2).broadcast_to([128, 64, 8]), op=ALU.add), reads=[BRT, BSM], writes=[BRT])
    for t in range(8):
        P.op("dve", I("max", out=TOP, in_=MK[:, t, :]), reads=[BRT, BSM], writes=[BSM])
        P.op("dve", I("tensor_copy", out=THR[:, 8 + t:9 + t], in_=TOP[:, 7:8]), reads=[BSM], writes=[BSM])
    SEL = RT[3]
    P.op("dve", I("tensor_tensor", out=SEL, in0=MK, in1=THR[:, 8:16].unsqueeze(2).broadcast_to([128, 8, NE]), op=ALU.is_ge), reads=[BRT, BSM], writes=[BRT])
    P.op("dve", I("tensor_tensor", out=SEL, in0=SEL, in1=SC, op=ALU.mult), reads=[BRT, BSC], writes=[BRT])
    P.op("dve", I("tensor_reduce", out=WS_[:, 0:8], in_=SEL, axis=AX.X, op=ALU.add), reads=[BRT], writes=[BSM])
    P.op("dve", I("reciprocal", out=WS_[:, 8:16], in_=WS_[:, 0:8]), reads=[BSM], writes=[BSM])
    P.op("dve", I("tensor_tensor", out=wdense, in0=SEL, in1=WS_[:, 8:16].unsqueeze(2).broadcast_to([128, 8, NE]), op=ALU.mult), reads=[BRT, BSM], writes=[Bwd])
    P.op("dve", I("tensor_scalar", out=wdense, in0=wdense, scalar1=2.5, scalar2=None, op0=ALU.mult), reads=[Bwd], writes=[Bwd])
    dump("d_H2T", H2T.rearrange("p a b -> p (a b)"), [128, 16 * 1024], BF16, flat(BH2))
    dump("d_WD", wdense.rearrange("p a b -> p (a b)"), [128, 8 * NE], F32, [Bwd])
    dump("d_SC", SC.rearrange("p a b -> p (a b)"), [128, 8 * NE], F32, [BSC])
    if stop_after <= 8:
        return finish()

    BRING = [Buf("ring%d" % i) for i in range(4)]
    RING = [carve(R2 + i * 16 * KB, [8192], BF16, BRING[i]) for i in range(4)]
    BAT = [[[Buf("at%d_%d_%d" % (p, fc, tb)) for tb in range(2)] for fc in range(4)] for p in range(2)]
    AT = [carve(192 * KB, [4, NOWN], BF16, flat(BAT[0])), carve(16 * KB, [4, NOWN], BF16, flat(BAT[1]))]
    NEXP = NE + 1

    def wsrc(e_, m):
        if e_ < NE:
            return (exp_gate[e_], exp_up[e_], exp_down[e_])[m]
        return (sh_gate, sh_up, sh_down)[m]

    blocks = [(e_, m) for e_ in range(NEXP) for m in range(3)]

    def issue(k):
        e_, m = blocks[k]
        s = k % 4
        nchunk = 16 if m < 2 else 4
        dst = RING[s].rearrange("p (c f) -> p c f", c=nchunk)
        src = wsrc(e_, m).rearrange("(c p) f -> p c f", p=128)
        P.dma("pool", I("dma_start", out=dst, in_=src), writes=[BRING[s]], sem="ring%d" % s)
        if m == 2:
            P.op("pool", I("tensor_tensor", out=dst, in0=dst, in1=G2.unsqueeze(1).broadcast_to([128, 4, D]), op=ALU.mult),
                 reads=[BG2], writes=[BRING[s]])

    for k in range(3):
        issue(k)
    for e_ in range(NEXP):
        par = e_ % 2
        kg, ku, kd = 3 * e_, 3 * e_ + 1, 3 * e_ + 2
        if kg + 3 < len(blocks):
            issue(kg + 3)
        for (kk, is_gate) in ((kg, True), (ku, False)):
            if not is_gate and ku + 3 < len(blocks):
                issue(ku + 3)
            W = RING[kk % 4].rearrange("p (c f) -> p c f", c=16)
            for fc in range(4):
                for tb in range(2):
                    bk = (fc * 2 + tb) % 4
                    fns = [I("matmul", ps(bk), lhsT=W[:, c, fc * 128:(fc + 1) * 128], rhs=H2T[:, c, tb * 512:(tb + 1) * 512],
                             start=(c == 0), stop=(c == 15)) for c in range(16)]
                    P.op("pe", fns, reads=[BRING[kk % 4]] + [b for t in range(4 * tb, 4 * tb + 4) for b in BH2[t]], writes=[PSB[bk]])
                    av = AT[par][:, fc, tb * 512:(tb + 1) * 512]
                    if is_gate:
                        P.op("act", I("activation", out=av, in_=ps(bk), func=AF.Silu), reads=[PSB[bk]], writes=[BAT[par][fc][tb]])
                    else:
                        P.op("dve", I("tensor_tensor", out=av, in0=ps(bk), in1=av, op=ALU.mult), reads=[PSB[bk], BAT[par][fc][tb]], writes=[BAT[par][fc][tb]])
        if kd + 3 < len(blocks):
            issue(kd + 3)
        WD = RING[kd % 4].rearrange("p (c f) -> p c f", c=4)
        for t in range(8):
            for db in range(4):
                bk = 4 + db
                fns = [I("matmul", ps(bk), lhsT=AT[par][:, fc, t * 128:(t + 1) * 128], rhs=WD[:, fc, db * 512:(db + 1) * 512],
                         start=(fc == 0), stop=(fc == 3)) for fc in range(4)]
                P.op("pe", fns, reads=[BRING[kd % 4]] + [BAT[par][fc][t // 4] for fc in range(4)], writes=[PSB[bk]])
                xv = X1[:, t, db * 512:(db + 1) * 512]
                if e_ < NE:
                    P.op("dve", I("scalar_tensor_tensor", out=xv, in0=ps(bk), scalar=wdense[:, t, e_:e_ + 1], in1=xv, op0=ALU.mult, op1=ALU.add),
                         reads=[PSB[bk], Bwd], writes=[BX1[t][db]])
                else:
                    P.op("dve", I("tensor_tensor", out=xv, in0=ps(bk), in1=xv, op=ALU.add), reads=[PSB[bk]], writes=[BX1[t][db]])
    for t in range(8):
        b = Buf("out%d" % t)
        P.dma("sp", I("dma_start", out=yout[t * 128:(t + 1) * 128, :], in_=X1[:, t, :]), reads=BX1[t], writes=[b], sem="out")
        OUTB.append(b)
    return finish()


_CACHE = {}


def _const_tables():
    if "tabs" in _CACHE:
        return _CACHE["tabs"]
    bf = ml_dtypes.bfloat16
    n = np.arange(S, dtype=np.int64)
    tabs = {}
    tabs["identb"] = np.eye(128, dtype=np.float32).astype(bf)
    tabs["identf"] = np.eye(128, dtype=np.float32)
    tabs["onesb"] = np.ones((128, 128), dtype=np.float32).astype(bf)
    c = np.arange(128, dtype=np.int64)
    ang = 2.0 * np.pi * ((c[:, None] * c[None, :]) % 128) / 128.0
    sc = 1.0 / np.sqrt(float(S) * 128.0)
    tabs["chC"] = (np.cos(ang) * sc).astype(np.float32).astype(bf)
    tabs["chS"] = (-np.sin(ang) * sc).astype(np.float32).astype(bf)
    inv = (10000.0 ** (-np.arange(32, dtype=np.float32) / 32.0)).astype(np.float32)
    pos = np.stack([n // 64, n % 64], axis=-1).astype(np.float32)
    a = (pos[:, :, None] * inv[None, None, :]).astype(np.float32)
    tabs["cos"] = np.cos(a).astype(np.float32).reshape(S, 64)
    tabs["sin"] = np.sin(a).astype(np.float32).reshape(S, 64)
    _CACHE["tabs"] = tabs
    return tabs


def _core_order(j):
    own = np.arange(j * NOWN, (j + 1) * NOWN)
    rest = np.concatenate([np.arange(0, j * NOWN), np.arange((j + 1) * NOWN, S)])
    return np.concatenate([own, rest]).astype(np.int64)


def prepare_inputs(inp):
    bf = ml_dtypes.bfloat16
    tabs = _const_tables()
    f32 = lambda a: np.ascontiguousarray(np.asarray(a, dtype=np.float32))
    x = f32(inp["x"])
    c = f32(inp["c"])
    ctx = f32(inp["ctx"])
    c_ctx = f32(inp["c_ctx"])
    shared = {
        "mod_w": f32(inp["mod_w"][0]),
        "mod_bT": f32(inp["mod_b"][0].reshape(96, 128).T),
        "mod_b": f32(inp["mod_b"][0].reshape(1, -1)),
        "norm1T": f32(inp["norm1_w"][0].reshape(16, 128).T),
        "norm2T": f32(inp["norm2_w"][0].reshape(16, 128).T),
        "w_in": f32(inp["w_in"][0]),
        "qnw": f32(inp["q_norm_w"][0].reshape(1, 128)),
        "knw": f32(inp["k_norm_w"][0].reshape(1, 128)),
        "w_fo": f32(inp["w_fourier_out"][0]),
        "w_ao": f32(inp["w_attn_out"][0]),
        "w_o": f32(inp["w_out"][0]),
        "router_w": f32(inp["router_w"][0]),
        "router_b": f32(inp["router_b"][0].reshape(1, NE)),
        "exp_gate": f32(inp["exp_gate"][0]),
        "exp_up": f32(inp["exp_up"][0]),
        "exp_down": f32(inp["exp_down"][0]),
        "sh_gate": f32(inp["shared_gate"][0]),
        "sh_up": f32(inp["shared_up"][0]),
        "sh_down": f32(inp["shared_down"][0]),
        "identb": tabs["identb"], "identf": tabs["identf"], "onesb": tabs["onesb"],
        "chC": tabs["chC"], "chS": tabs["chS"],
    }
    dft = {}
    for j in range(4):
        order = _core_order(j)
        k = np.arange(j * NOWN, (j + 1) * NOWN, dtype=np.int64)
        ang = (2.0 * np.pi / S) * ((order[:, None] * k[None, :]) % S).astype(np.float64)
        dft[j] = (np.cos(ang).astype(np.float32).astype(bf), np.sin(ang).astype(np.float32).astype(bf),
                  np.ascontiguousarray(tabs["cos"][order]), np.ascontiguousarray(tabs["sin"][order]), order)
    in_maps = []
    for core in range(8):
        b, j = core // 4, core % 4
        dC, dS, cs, sn, order = dft[j]
        cond = np.stack([c[b], c_ctx], axis=-1)
        condT = cond.reshape(16, 128, 2).transpose(1, 0, 2).reshape(128, 32)
        m = dict(shared)
        m.update({
            "xall": np.ascontiguousarray(x[b][order]),
            "ctxb": np.ascontiguousarray(ctx[b]),
            "condT": np.ascontiguousarray(condT),
            "cosA": cs, "sinA": sn, "dftC": dC, "dftS": dS,
        })
        in_maps.append(m)
    return in_maps


def kernel(**inputs):
    if "nc" not in _CACHE:
        _CACHE["nc"] = build_program()
    nc = _CACHE["nc"]
    in_maps = prepare_inputs(inputs)
    res = run_bass_kernel_spmd(nc, in_maps, core_ids=list(range(8)))
    out = np.empty((2, S, D), dtype=np.float32)
    for core in range(8):
        b, j = core // 4, core % 4
        out[b, j * NOWN:(j + 1) * NOWN, :] = res.results[core]["yout"]
    return out
```

```python
from contextlib import ExitStack
import numpy as np
import ml_dtypes
import concourse.bass as bass
import concourse.mybir as mybir
from concourse.bass_utils import run_bass_kernel_spmd

F32 = mybir.dt.float32
BF16 = mybir.dt.bfloat16
U8 = mybir.dt.uint8
AF = mybir.ActivationFunctionType
ALU = mybir.AluOpType
AX = mybir.AxisListType

D = 2048
S = 4096
NOWN = 1024
NCTX = 256
NE = 64
EPS = 1e-6
Q_OFF, K_OFF, V_OFF, GF_OFF, GA_OFF = 1024, 3072, 3584, 4096, 6144


class Buf:
    __slots__ = ("name", "lw", "rd", "excl")

    def __init__(self, name="", excl=False):
        self.name = name
        self.lw = None
        self.rd = {}
        self.excl = excl


class Prog:
    ENGS = ("pe", "act", "dve", "pool", "sp")

    def __init__(self, nc):
        self.nc = nc
        self.lists = {e: [] for e in self.ENGS}
        self.cnt = {e: 0 for e in self.ENGS}
        self.waited = {e: {} for e in self.ENGS}
        self.dma_sems = {}
        self.sem_handles = {}
        self._rec = None

    def rec(self, fn, *a, **k):
        self._rec = []
        fn(*a, **k)
        r, self._rec = self._rec, None
        return r

    def play(self, lists):
        idx = [0] * len(lists)
        left = sum(len(l) for l in lists)
        while left:
            for j, l in enumerate(lists):
                if idx[j] < len(l):
                    kind, a, k = l[idx[j]]
                    idx[j] += 1
                    left -= 1
                    getattr(self, kind)(*a, **k)

    def _deps(self, reads, writes):
        deps = []
        for b in reads:
            if b.lw is not None:
                deps.append(b.lw)
        for b in writes:
            if b.lw is not None:
                deps.append(b.lw)
            deps.extend(b.rd.items())
        return deps

    def _emit_waits(self, eng, deps):
        need = {}
        w = self.waited[eng]
        for (k, v) in deps:
            if v > w.get(k, 0) and v > need.get(k, 0):
                need[k] = v
        for k, v in need.items():
            w[k] = v
            self.lists[eng].append(("wait", k, v))

    def _commit(self, tok, reads, writes):
        k, v = tok
        for b in reads:
            if b.rd.get(k, 0) < v:
                b.rd[k] = v
        for b in writes:
            b.lw = tok
            b.rd = {}

    def op(self, eng, fns, reads=(), writes=()):
        if self._rec is not None:
            self._rec.append(("op", (eng, fns, list(reads), list(writes)), {}))
            return None
        if not isinstance(fns, (list, tuple)):
            fns = [fns]
        ex = [b for b in reads if b.excl]
        if ex:
            writes = list(writes) + ex
        self._emit_waits(eng, self._deps(reads, writes))
        self.cnt[eng] += 1
        tok = ("e_" + eng, self.cnt[eng])
        L = self.lists[eng]
        for f in fns[:-1]:
            L.append(("ins", f, None))
        L.append(("ins", fns[-1], ("e_" + eng, 1)))
        self._commit(tok, reads, writes)
        return tok

    def dma(self, eng, fn, reads=(), writes=(), sem=None):
        if self._rec is not None:
            self._rec.append(("dma", (eng, fn, list(reads), list(writes), sem), {}))
            return None
        self._emit_waits(eng, self._deps(reads, writes))
        sem = "d_" + sem
        self.dma_sems[sem] = self.dma_sems.get(sem, 0) + 16
        tok = (sem, self.dma_sems[sem])
        self.lists[eng].append(("ins", fn, (sem, 16)))
        self._commit(tok, reads, writes)
        return tok

    def wait_all(self, eng, bufs):
        deps = [b.lw for b in bufs if b.lw is not None]
        self._emit_waits(eng, deps)

    def emit(self, stack):
        nc = self.nc
        keys = ["e_" + e for e in self.ENGS] + sorted(self.dma_sems.keys())
        for k in keys:
            self.sem_handles[k] = stack.enter_context(nc.semaphore(k))
        block = stack.enter_context(nc.Block())
        H = self.sem_handles

        def run(e, items):
            for it in items:
                if it[0] == "wait":
                    e.wait_ge(H[it[1]], it[2])
                else:
                    ins = it[1](e)
                    if it[2] is not None:
                        ins.then_inc(H[it[2][0]], it[2][1])

        @block.tensor
        def _(e):
            run(e, self.lists["pe"])

        @block.scalar
        def _(e):
            run(e, self.lists["act"])

        @block.vector
        def _(e):
            run(e, self.lists["dve"])

        @block.gpsimd
        def _(e):
            run(e, self.lists["pool"])

        @block.sync
        def _(e):
            run(e, self.lists["sp"])


def I(method, *args, **kw):
    return lambda e: getattr(e, method)(*args, **kw)


def build_program(debug=False, stop_after=99):
    nc = bass.Bass("TRN2", target_bir_lowering=False)

    def din(name, shape, dt=F32):
        return nc.dram_tensor(name, list(shape), dt, kind="ExternalInput").ap()

    xall = din("xall", [S, D])
    ctxb = din("ctxb", [NCTX, D])
    condT = din("condT", [128, 32])
    mod_w = din("mod_w", [D, 6 * D])
    mod_bT = din("mod_bT", [128, 96])
    mod_b = din("mod_b", [1, 6 * D])
    norm1T = din("norm1T", [128, 16])
    norm2T = din("norm2T", [128, 16])
    w_in = din("w_in", [D, 8192])
    qnw = din("qnw", [1, 128])
    knw = din("knw", [1, 128])
    w_fo = din("w_fo", [1024, D])
    w_ao = din("w_ao", [D, D])
    w_o = din("w_o", [D, D])
    router_w = din("router_w", [D, NE])
    router_b = din("router_b", [1, NE])
    exp_gate = din("exp_gate", [NE, D, 512])
    exp_up = din("exp_up", [NE, D, 512])
    exp_down = din("exp_down", [NE, 512, D])
    sh_gate = din("sh_gate", [D, 512])
    sh_up = din("sh_up", [D, 512])
    sh_down = din("sh_down", [512, D])
    identb_d = din("identb", [128, 128], BF16)
    identf_d = din("identf", [128, 128])
    onesb_d = din("onesb", [128, 128], BF16)
    chC_d = din("chC", [128, 128], BF16)
    chS_d = din("chS", [128, 128], BF16)
    cosA = din("cosA", [S, 64])
    sinA = din("sinA", [S, 64])
    dftC = din("dftC", [S, NOWN], BF16)
    dftS = din("dftS", [S, NOWN], BF16)
    yout = nc.dram_tensor("yout", [NOWN, D], F32, kind="ExternalOutput").ap()
    dbg = {}

    st = ExitStack()
    KB = 1024
    arena = st.enter_context(nc.sbuf_tensor("arena", [128, 206 * KB], U8))
    psf = [st.enter_context(nc.psum_tensor("ps%d" % i, [128, 512], F32)) for i in range(8)]
    PSB = [Buf("ps%d" % i, excl=True) for i in range(8)]
    P = Prog(nc)

    occupants = []

    def guard(bufs, guards):
        for b in bufs:
            for g in guards:
                if g.lw is not None and b.rd.get(g.lw[0], 0) < g.lw[1]:
                    b.rd[g.lw[0]] = g.lw[1]
                for k, v in g.rd.items():
                    if b.rd.get(k, 0) < v:
                        b.rd[k] = v

    def carve(off, dims, dt, bufs=None):
        n = int(np.prod(dims))
        sz = 4 if dt == F32 else 2
        end = off + n * sz
        assert end <= 206 * KB, (off, dims)
        ap = arena[:, off:end].bitcast(dt)
        if len(dims) == 2:
            ap = ap.rearrange("p (a b) -> p a b", a=dims[0])
        elif len(dims) == 3:
            ap = ap.rearrange("p (a b c) -> p a b c", a=dims[0], b=dims[1])
        if bufs is not None:
            if isinstance(bufs, Buf):
                bufs = [bufs]
            bufs = list(bufs)
            keep = []
            for (o2, e2, b2) in occupants:
                if o2 < end and off < e2:
                    if b2 not in bufs:
                        guard(bufs, [b2])
                    if not (off <= o2 and e2 <= end):
                        keep.append((o2, e2, b2))
                else:
                    keep.append((o2, e2, b2))
            occupants[:] = keep
            for b in bufs:
                occupants.append((off, end, b))
        return ap

    def ps(i):
        return psf[i][:]

    def psb(i):
        return psf[i][:].bitcast(BF16)

    DBGB = []
    OUTB = []

    def dump(name, ap_sb, shape, dt, bufs):
        if not debug:
            return
        d = nc.dram_tensor(name, list(shape), dt, kind="ExternalOutput").ap()
        dbg[name] = d
        b = Buf("dbg_" + name)
        P.dma("sp", I("dma_start", out=d, in_=ap_sb), reads=bufs, writes=[b], sem="dbg")
        DBGB.append(b)

    def finish():
        P.wait_all("sp", DBGB + OUTB)
        P.emit(st)
        st.close()
        return nc

    def flat(bb):
        return [b for x in bb for b in x]

    Bc = Buf("consts")
    Bmod = Buf("mod")
    Bstat = [Buf("stat%d" % i) for i in range(4)]
    Bwd = Buf("wdense")
    o = [0]

    def cst(dims, dt, buf):
        ap = carve(o[0], dims, dt, buf)
        o[0] += int(np.prod(dims)) * (4 if dt == F32 else 2)
        o[0] = (o[0] + 31) // 32 * 32
        return ap

    identb = cst([128], BF16, Bc)
    identf = cst([128], F32, Bc)
    onesb = cst([128], BF16, Bc)
    chC = cst([128], BF16, Bc)
    chS = cst([128], BF16, Bc)
    qnw_bc = cst([128], F32, Bc)
    knw_bc = cst([128], F32, Bc)
    cond_f = cst([32], F32, Bc)
    modbT = cst([96], F32, Bc)
    n1T = cst([16], F32, Bc)
    n2T = cst([16], F32, Bc)
    rb_bc = cst([NE], F32, Bc)
    scT = cst([16, 2], BF16, Bmod)
    scT_rep = cst([16, 128], BF16, Bmod)
    modA = cst([32, 2], F32, Bmod)
    S1 = cst([16, 2], F32, Bmod)
    modB = cst([32, 2], F32, Bmod)
    S2 = cst([16], F32, Bmod)
    B2 = cst([16], F32, Bmod)
    wdense = cst([8, NE], F32, Bwd)
    stat = [cst([4], F32, Bstat[i]) for i in range(4)]
    assert o[0] <= 12 * KB, o[0]

    YFT_OFF = 12 * KB
    R1 = 28 * KB
    R2 = 60 * KB
    R3 = 128 * KB

    for (dst, src) in ((identb, identb_d[:, :]), (identf, identf_d[:, :]), (onesb, onesb_d[:, :]), (chC, chC_d[:, :]),
                       (chS, chS_d[:, :]), (qnw_bc, qnw[0:1, :].partition_broadcast(128)),
                       (knw_bc, knw[0:1, :].partition_broadcast(128)), (cond_f, condT[:, :]), (modbT, mod_bT[:, :]),
                       (n1T, norm1T[:, :]), (n2T, norm2T[:, :]), (rb_bc, router_b[0:1, :].partition_broadcast(128))):
        P.dma("sp", I("dma_start", out=dst, in_=src), writes=[Bc], sem="c0")

    P.op("act", I("activation", out=scT.rearrange("p a b -> p (a b)"), in_=cond_f, func=AF.Silu), reads=[Bc], writes=[Bmod])
    P.op("dve", I("tensor_copy", out=scT_rep, in_=scT[:, :, 0:1].broadcast_to([128, 16, 128])), reads=[Bmod], writes=[Bmod])

    BMS = [Buf("ms0"), Buf("ms1")]
    mcount = [0]

    def mod_cols(col0, ncols, slot_off, psbank, pscol0, row_form, evac, bw=256, defer=False, dma_reads=()):
        nblk = ncols // bw
        slots = [carve(slot_off + i * bw * 32, [16, bw], BF16, BMS[i]) for i in range(2)]
        def blk(j):
            s = mcount[0] % 2
            mcount[0] += 1
            c0 = col0 + bw * j
            src = mod_w[:, c0:c0 + bw].rearrange("(c p) f -> p c f", p=128)
            P.dma("pool", I("dma_start", out=slots[s], in_=src), reads=list(dma_reads), writes=[BMS[s]], sem="ms%d" % s)
            if not row_form:
                for fc in range(bw // 128):
                    ci = (bw // 128) * j + fc
                    outap = ps(psbank)[:, pscol0 + 2 * ci: pscol0 + 2 * ci + 2]
                    fns = [I("matmul", outap, lhsT=slots[s][:, c, fc * 128:(fc + 1) * 128], rhs=scT[:, c, :],
                             start=(c == 0), stop=(c == 15)) for c in range(16)]
                    P.op("pe", fns, reads=[BMS[s], Bmod], writes=[PSB[psbank]])
            else:
                bk = psbank + (j % 2)
                fns = [I("matmul", ps(bk)[:, 0:256], lhsT=scT_rep[:, c, :], rhs=slots[s][:, c, :],
                         start=(c == 0), stop=(c == 15)) for c in range(16)]
                P.op("pe", fns, reads=[BMS[s], Bmod], writes=[PSB[bk]])
                evac(j, bk)

        thunks = [(lambda j: lambda: blk(j))(j) for j in range(nblk)]
        if defer:
            return thunks
        for th in thunks:
            th()

    mod_cols(0, 4096, R3, 7, 0, False, None, bw=512, dma_reads=[Bc])
    P.op("dve", I("tensor_tensor", out=modA, in0=ps(7)[:, 0:64].rearrange("p (a b) -> p a b", b=2),
                  in1=modbT[:, 0:32].unsqueeze(2).broadcast_to([128, 32, 2]), op=ALU.add), reads=[PSB[7], Bc], writes=[Bmod])
    P.op("dve", I("tensor_scalar", out=S1, in0=modA[:, 16:32, :], scalar1=1.0, scalar2=None, op0=ALU.add), reads=[Bmod], writes=[Bmod])
    P.op("dve", I("tensor_tensor", out=S1, in0=S1, in1=n1T.unsqueeze(2).broadcast_to([128, 16, 2]), op=ALU.mult),
         reads=[Bmod, Bc], writes=[Bmod])

    def nt_load(src_rows, xt, Bx):
        P.dma("sp", I("dma_start", out=xt, in_=src_rows), writes=[Bx], sem=Bx.name)

    def norm_transpose_tile(src_rows, r, xt, xn, si, dst_fn, dst_bufs, Bx, Bxn):
        nt_load(src_rows, xt, Bx)
        nt_compute(r, xt, xn, si, dst_fn, dst_bufs, Bx, Bxn)

    def nt_compute(r, xt, xn, si, dst_fn, dst_bufs, Bx, Bxn, bank0=0):
        stt, Bst = stat[si], Bstat[si]
        P.op("pool", I("memset", stt[:, 0:1], 0.0), writes=[Bst])
        P.op("act", I("activation", out=xn, in_=xt, func=AF.Square, accum_out=stt[:, 0:1]), reads=[Bx], writes=[Bxn, Bst])
        P.op("act", I("activation", out=stt[:, 1:2], in_=stt[:, 0:1], func=AF.Sqrt, scale=1.0 / D, bias=EPS), reads=[Bst], writes=[Bst])
        P.op("dve", I("reciprocal", out=stt[:, 2:3], in_=stt[:, 1:2]), reads=[Bst], writes=[Bst])
        P.op("dve", I("tensor_scalar", out=xn, in0=xt, scalar1=stt[:, 2:3], scalar2=None, op0=ALU.mult), reads=[Bx, Bst], writes=[Bxn])
        for g in range(4):
            bk = bank0 + g % 2
            pv = psb(bk)[:, 0:512]
            fns = [I("transpose", pv[:, j * 128:(j + 1) * 128], xn[:, (4 * g + j) * 128:(4 * g + j + 1) * 128], identb) for j in range(4)]
            P.op("pe", fns, reads=[Bxn, Bc], writes=[PSB[bk]])
            if g % 2 == 0:
                fns = [I("activation", out=dst_fn(4 * g + j), in_=pv[:, j * 128:(j + 1) * 128], func=AF.Identity,
                         scale=S1[:, 4 * g + j, r:r + 1], bias=modA[:, 4 * g + j, r:r + 1]) for j in range(4)]
                P.op("act", fns, reads=[PSB[bk], Bmod], writes=[dst_bufs[g]])
            else:
                fns = [I("tensor_scalar", out=dst_fn(4 * g + j), in0=pv[:, j * 128:(j + 1) * 128], scalar1=S1[:, 4 * g + j, r:r + 1],
                         scalar2=modA[:, 4 * g + j, r:r + 1], op0=ALU.mult, op1=ALU.add) for j in range(4)]
                P.op("dve", fns, reads=[PSB[bk], Bmod], writes=[dst_bufs[g]])

    dump("d_S1", S1.rearrange("p a b -> p (a b)"), [128, 32], F32, [Bmod])
    dump("d_modA", modA.rearrange("p a b -> p (a b)"), [128, 64], F32, [Bmod])
    if stop_after <= 0:
        return finish()

    BWU = [Buf("wu0"), Buf("wu1")]
    WU = carve(R1, [16, 1024], BF16, BWU)
    for h in range(2):
        src = w_in[:, h * 512:(h + 1) * 512].rearrange("(c p) f -> p c f", p=128)
        P.dma("pool", I("dma_start", out=WU[:, :, h * 512:(h + 1) * 512], in_=src), writes=[BWU[h]], sem="wu%d" % h)
    BU = [[Buf("u%d_%d" % (t, n)) for n in range(2)] for t in range(32)]
    U = carve(R2, [32, 1024], BF16, flat(BU))
    BXS = [Buf("xs0"), Buf("xs1")]
    BXN = [Buf("xn0"), Buf("xn1")]
    BHT = [[Buf("ht%d_%d" % (s, g)) for g in range(4)] for s in range(2)]
    XS = [carve(R3 + i * 8 * KB, [D], F32, BXS[i]) for i in range(2)]
    XN = [carve(R3 + 16 * KB + i * 4 * KB, [D], BF16, BXN[i]) for i in range(2)]
    HT = [carve(R3 + 24 * KB + i * 4 * KB, [16, 128], BF16, BHT[i]) for i in range(2)]
    def s1_A(t):
        s = t % 2
        nt_compute(0, XS[s], XN[s], s, (lambda s: lambda c: HT[s][:, c, :])(s), BHT[s], BXS[s], BXN[s])

    def s1_Bmm(t):
        s = t % 2
        for n in range(2):
            bk = 2 + (2 * t + n) % 4
            fns = [I("matmul", ps(bk), lhsT=HT[s][:, c, :], rhs=WU[:, c, n * 512:(n + 1) * 512], start=(c == 0), stop=(c == 15)) for c in range(16)]
            P.op("pe", fns, reads=BHT[s] + [BWU[n]], writes=[PSB[bk]])

    def s1_Bev(t):
        for n in range(2):
            bk = 2 + (2 * t + n) % 4
            if n == 0:
                P.op("act", I("activation", out=U[:, t, n * 512:(n + 1) * 512], in_=ps(bk), func=AF.Identity), reads=[PSB[bk]], writes=[BU[t][n]])
            else:
                P.op("dve", I("tensor_copy", out=U[:, t, n * 512:(n + 1) * 512], in_=ps(bk)), reads=[PSB[bk]], writes=[BU[t][n]])

    nt_load(xall[0:128, :], XS[0], BXS[0])
    for i in range(33):
        if i + 1 < 32:
            nt_load(xall[(i + 1) * 128:(i + 2) * 128, :], XS[(i + 1) % 2], BXS[(i + 1) % 2])
        bm = P.rec(s1_Bmm, i - 1) if i >= 1 else []
        a = P.rec(s1_A, i) if i < 32 else []
        P.play([bm[0:1] + a[0:9] + bm[1:2] + a[9:]])
        if i >= 1:
            s1_Bev(i - 1)
    dump("d_U", U.rearrange("p a b -> p (a b)"), [128, 32 * 1024], BF16, flat(BU))
    if stop_after <= 1:
        return finish()

    BTAB = [Buf("tabc"), Buf("tabs")]
    BABT = [[Buf("abt%d%d" % (i, j)) for j in range(2)] for i in range(2)]
    BYF = [[Buf("yf%d_%d" % (g, kb)) for kb in range(2)] for g in range(8)]
    YFT = carve(YFT_OFF, [8, NOWN], BF16, flat(BYF))
    TABC = carve(R3, [32, 512], BF16, BTAB[0])
    TABS = carve(R3 + 32 * KB, [32, 512], BF16, BTAB[1])
    ABT = [[carve(R3 + 64 * KB + (2 * i + j) * KB, [512], BF16, BABT[i][j]) for j in range(2)] for i in range(2)]
    for kb in range(2):
        for (tab, srcd, Bt, nm) in ((TABC, dftC, BTAB[0], "tabc"), (TABS, dftS, BTAB[1], "tabs")):
            for hf in range(2):
                src = srcd[hf * 2048:(hf + 1) * 2048, kb * 512:(kb + 1) * 512].rearrange("(t p) k -> p t k", p=128)
                P.dma("sp", I("dma_start", out=tab[:, hf * 16:(hf + 1) * 16, :], in_=src), writes=[Bt], sem=nm)
        for g in range(8):
            i = g % 2
            ba, bb_, by = 0 + i, 2 + i, 4 + i
            ub = [BU[t][g // 4] for t in range(32)]
            fns = [I("matmul", ps(ba), lhsT=U[:, t, g * 128:(g + 1) * 128], rhs=TABC[:, t, :], start=(t == 0), stop=(t == 31)) for t in range(32)]
            P.op("pe", fns, reads=ub + [BTAB[0]], writes=[PSB[ba]])
            fns = [I("matmul", ps(bb_), lhsT=U[:, t, g * 128:(g + 1) * 128], rhs=TABS[:, t, :], start=(t == 0), stop=(t == 31)) for t in range(32)]
            P.op("pe", fns, reads=ub + [BTAB[1]], writes=[PSB[bb_]])
            P.op("act", I("activation", out=ABT[i][0], in_=ps(ba), func=AF.Identity), reads=[PSB[ba]], writes=[BABT[i][0]])
            P.op("dve", I("tensor_copy", out=ABT[i][1], in_=ps(bb_)), reads=[PSB[bb_]], writes=[BABT[i][1]])
            fns = [I("matmul", ps(by), lhsT=chC, rhs=ABT[i][0], start=True, stop=False),
                   I("matmul", ps(by), lhsT=chS, rhs=ABT[i][1], start=False, stop=True)]
            P.op("pe", fns, reads=[BABT[i][0], BABT[i][1], Bc], writes=[PSB[by]])
            if i == 0:
                P.op("act", I("activation", out=YFT[:, g, kb * 512:(kb + 1) * 512], in_=ps(by), func=AF.Identity), reads=[PSB[by]], writes=[BYF[g][kb]])
            else:
                P.op("dve", I("tensor_copy", out=YFT[:, g, kb * 512:(kb + 1) * 512], in_=ps(by)), reads=[PSB[by]], writes=[BYF[g][kb]])
    dump("d_YFT", YFT.rearrange("p a b -> p (a b)"), [128, 8 * 1024], BF16, flat(BYF))
    if stop_after <= 2:
        return finish()

    BHTO = [[Buf("hto%d_%d" % (t, g)) for g in range(4)] for t in range(8)]
    HTO = carve(R1, [16, NOWN], BF16, flat(BHTO))
    BKT = [Buf("kt%d" % t) for t in range(34)]
    BVS = [Buf("vs%d" % t) for t in range(34)]
    KT = carve(R2, [4, 4352], BF16, BKT)
    VS = carve(R2 + 34 * KB, [34, 512], BF16, BVS)
    BWKV = [Buf("wkv0"), Buf("wkv1")]
    WKV = carve(R3, [16, 1024], BF16, BWKV)
    for h in range(2):
        src = w_in[:, K_OFF + h * 512:K_OFF + (h + 1) * 512].rearrange("(c p) f -> p c f", p=128)
        P.dma("pool", I("dma_start", out=WKV[:, :, h * 512:(h + 1) * 512], in_=src), writes=[BWKV[h]], sem="wkv%d" % h)
    o3 = R3 + 32 * KB
    BXS = [Buf("xs0b"), Buf("xs1b")]
    BXN = [Buf("xn0b"), Buf("xn1b")]
    BHT = [[Buf("htb%d_%d" % (s, g)) for g in range(4)] for s in range(2)]
    XS = [carve(o3 + i * 8 * KB, [D], F32, BXS[i]) for i in range(2)]
    XN = [carve(o3 + 16 * KB + i * 4 * KB, [D], BF16, BXN[i]) for i in range(2)]
    HT = [carve(o3 + 24 * KB + i * 4 * KB, [16, 128], BF16, BHT[i]) for i in range(2)]
    o3 += 32 * KB
    BSQ, BKN, BT12, BT34, BKR, BSSK = Buf("sq"), Buf("kn"), Buf("t12"), Buf("t34"), Buf("kr"), Buf("ssk")
    BCS = [Buf("cs0"), Buf("cs1")]
    SQ = carve(o3, [512], F32, BSQ)
    KN = carve(o3 + 2 * KB, [512], F32, BKN)
    T1 = carve(o3 + 4 * KB, [256], F32, BT12)
    T2 = carve(o3 + 5 * KB, [256], F32, BT12)
    T3 = carve(o3 + 6 * KB, [256], F32, BT34)
    T4 = carve(o3 + 7 * KB, [256], F32, BT34)
    KR = carve(o3 + 8 * KB, [512], BF16, BKR)
    CS = [carve(o3 + 9 * KB + i * 512, [128], F32, BCS[i]) for i in range(2)]
    SSK = carve(o3 + 10 * KB, [16], F32, BSSK)

    def qk_norm_rope(psbank, nw_bc, rope_slot, out_bf):
        Bp = PSB[psbank]
        h4 = lambda ap: ap.rearrange("p (h d) -> p h d", h=4)
        P.op("act", I("activation", out=SQ, in_=ps(psbank), func=AF.Square), reads=[Bp], writes=[BSQ])
        P.op("dve", I("tensor_reduce", out=SSK[:, 0:4], in_=h4(SQ), axis=AX.X, op=ALU.add), reads=[BSQ], writes=[BSSK])
        P.op("act", I("activation", out=SSK[:, 4:8], in_=SSK[:, 0:4], func=AF.Sqrt, scale=1.0 / 128, bias=EPS), reads=[BSSK], writes=[BSSK])
        P.op("dve", I("reciprocal", out=SSK[:, 8:12], in_=SSK[:, 4:8]), reads=[BSSK], writes=[BSSK])
        P.op("dve", I("tensor_tensor", out=h4(KN), in0=h4(ps(psbank)), in1=nw_bc.unsqueeze(1).broadcast_to([128, 4, 128]), op=ALU.mult),
             reads=[Bp, Bc], writes=[BKN])
        rbc = SSK[:, 8:12].unsqueeze(2).broadcast_to([128, 4, 128])
        if rope_slot is None:
            P.op("dve", I("tensor_tensor", out=h4(out_bf), in0=h4(KN), in1=rbc, op=ALU.mult), reads=[BKN, BSSK], writes=[BKR])
            return
        P.op("dve", I("tensor_tensor", out=h4(KN), in0=h4(KN), in1=rbc, op=ALU.mult), reads=[BKN, BSSK], writes=[BKN])
        kn5 = KN.rearrange("p (h x y f) -> p h x y f", h=4, x=2, y=2)
        xa, xb = kn5[:, :, :, 0, :], kn5[:, :, :, 1, :]
        o5 = out_bf.rearrange("p (h x y f) -> p h x y f", h=4, x=2, y=2)
        oa, ob = o5[:, :, :, 0, :], o5[:, :, :, 1, :]
        cs = CS[rope_slot]
        cb = cs[:, 0:64].rearrange("p (x f) -> p x f", x=2).unsqueeze(1).broadcast_to([128, 4, 2, 32])
        sb = cs[:, 64:128].rearrange("p (x f) -> p x f", x=2).unsqueeze(1).broadcast_to([128, 4, 2, 32])
        v4 = lambda ap: ap.rearrange("p (h x f) -> p h x f", h=4, x=2)
        Bcs = BCS[rope_slot]
        P.op("dve", [I("tensor_tensor", out=v4(T1), in0=xa, in1=cb, op=ALU.mult), I("tensor_tensor", out=v4(T2), in0=xb, in1=sb, op=ALU.mult)],
             reads=[BKN, Bcs], writes=[BT12])
        P.op("pool", [I("tensor_tensor", out=v4(T3), in0=xb, in1=cb, op=ALU.mult), I("tensor_tensor", out=v4(T4), in0=xa, in1=sb, op=ALU.mult)],
             reads=[BKN, Bcs], writes=[BT34])
        P.op("dve", I("tensor_tensor", out=oa, in0=v4(T1), in1=v4(T2), op=ALU.subtract), reads=[BT12], writes=[BKR])
        P.op("pool", I("tensor_tensor", out=ob, in0=v4(T3), in1=v4(T4), op=ALU.add), reads=[BT34, BKR], writes=[BKR])

    def load_cs(slot, t):
        P.dma("sp", I("dma_start", out=CS[slot][:, 0:64], in_=cosA[t * 128:(t + 1) * 128, :]), writes=[BCS[slot]], sem="cs%d" % slot)
        P.dma("sp", I("dma_start", out=CS[slot][:, 64:128], in_=sinA[t * 128:(t + 1) * 128, :]), writes=[BCS[slot]], sem="cs%d" % slot)

    def s3_tile(t):
        s = t % 2
        if t < 8:
            return (lambda t: lambda c: HTO[:, c, t * 128:(t + 1) * 128])(t), BHTO[t]
        return (lambda s: lambda c: HT[s][:, c, :])(s), BHT[s]

    def s3_rows(t):
        return xall[t * 128:(t + 1) * 128, :] if t < 32 else ctxb[(t - 32) * 128:(t - 31) * 128, :]

    def s3_A(t):
        s = t % 2
        dst_fn, dbufs = s3_tile(t)
        nt_compute(0 if t < 32 else 1, XS[s], XN[s], s, dst_fn, dbufs, BXS[s], BXN[s])

    def s3_B(t):
        s = t % 2
        dst_fn, dbufs = s3_tile(t)
        bkk, bkv = 2 + s, 4 + s
        fns = [I("matmul", ps(bkk), lhsT=dst_fn(c), rhs=WKV[:, c, 0:512], start=(c == 0), stop=(c == 15)) for c in range(16)]
        P.op("pe", fns, reads=dbufs + [BWKV[0]], writes=[PSB[bkk]])
        fns = [I("matmul", ps(bkv), lhsT=dst_fn(c), rhs=WKV[:, c, 512:1024], start=(c == 0), stop=(c == 15)) for c in range(16)]
        P.op("pe", fns, reads=dbufs + [BWKV[1]], writes=[PSB[bkv]])

    def s3_Bev(t):
        bkv = 4 + t % 2
        P.op("act", I("activation", out=VS[:, t, :], in_=ps(bkv), func=AF.Identity), reads=[PSB[bkv]], writes=[BVS[t]])

    def s3_C(t):
        s = t % 2
        qk_norm_rope(2 + s, knw_bc, s if t < 32 else None, KR)
        bt = 6 + s
        pv = psb(bt)[:, 0:512]
        fns = [I("transpose", pv[:, h * 128:(h + 1) * 128], KR[:, h * 128:(h + 1) * 128], identb) for h in range(4)]
        P.op("pe", fns, reads=[BKR, Bc], writes=[PSB[bt]])
        P.op("dve", I("tensor_copy", out=KT[:, :, t * 128:(t + 1) * 128], in_=pv.rearrange("p (h d) -> p h d", h=4)), reads=[PSB[bt]], writes=[BKT[t]])

    nt_load(s3_rows(0), XS[0], BXS[0])
    load_cs(0, 0)
    load_cs(1, 1)
    for i in range(36):
        if i + 1 < 34:
            nt_load(s3_rows(i + 1), XS[(i + 1) % 2], BXS[(i + 1) % 2])
        bm = P.rec(s3_B, i - 1) if 0 <= i - 1 < 34 else []
        a = P.rec(s3_A, i) if i < 34 else []
        c = P.rec(s3_C, i - 2) if 0 <= i - 2 < 34 else []
        P.play([bm[0:1]])
        P.play([a[0:9], c[0:9]])
        P.play([bm[1:2]])
        P.play([a[9:], c[9:]])
        if 0 <= i - 1 < 34:
            s3_Bev(i - 1)
        if 0 <= i - 2 < 34 and i < 32:
            load_cs(i % 2, i)
    dump("d_KT", KT.rearrange("p a b -> p (a b)"), [128, 4 * 4352], BF16, BKT)
    dump("d_VS", VS.rearrange("p a b -> p (a b)"), [128, 34 * 512], BF16, BVS)
    dump("d_HTO", HTO.rearrange("p a b -> p (a b)"), [128, 16 * 1024], BF16, flat(BHTO))
    if stop_after <= 3:
        return finish()

    BQT = [[Buf("qt%d_%d" % (qb, t)) for t in range(8)] for qb in range(4)]
    QT = carve(R3, [16, NOWN], BF16, flat(BQT))
    BWQ = [Buf("wq0"), Buf("wq1")]
    WQ = [carve(R3 + 32 * KB + i * 16 * KB, [16, 512], BF16, BWQ[i]) for i in range(2)]
    def wq_load(qblk):
        src = w_in[:, Q_OFF + qblk * 512:Q_OFF + (qblk + 1) * 512].rearrange("(c p) f -> p c f", p=128)
        P.dma("pool", I("dma_start", out=WQ[qblk % 2], in_=src), writes=[BWQ[qblk % 2]], sem="wq%d" % (qblk % 2))

    wq_load(0)
    for qblk in range(4):
        s = qblk % 2
        if qblk + 1 < 4:
            wq_load(qblk + 1)
        for t in range(8):
            cs_s = t % 2
            load_cs(cs_s, t)
            bq = 2 + (t % 2)
            fns = [I("matmul", ps(bq), lhsT=HTO[:, c, t * 128:(t + 1) * 128], rhs=WQ[s][:, c, :], start=(c == 0), stop=(c == 15)) for c in range(16)]
            P.op("pe", fns, reads=BHTO[t] + [BWQ[s]], writes=[PSB[bq]])
            qk_norm_rope(bq, qnw_bc, cs_s, KR)
            bt = 6 + (t % 2)
            pv = psb(bt)[:, 0:512]
            fns = [I("transpose", pv[:, h * 128:(h + 1) * 128], KR[:, h * 128:(h + 1) * 128], identb) for h in range(4)]
            P.op("pe", fns, reads=[BKR, Bc], writes=[PSB[bt]])
            P.op("dve", I("tensor_copy", out=QT[:, 4 * qblk:4 * qblk + 4, t * 128:(t + 1) * 128], in_=pv.rearrange("p (h d) -> p h d", h=4)),
                 reads=[PSB[bt]], writes=[BQT[qblk][t]])
    dump("d_QT", QT.rearrange("p a b -> p (a b)"), [128, 16 * 1024], BF16, flat(BQT))
    if stop_after <= 4:
        return finish()

    BYA = [[Buf("ya%d_%d" % (h, qb)) for qb in range(2)] for h in range(16)]
    YAT = carve(R1, [16, NOWN], BF16, flat(BYA))
    o5_ = R3 + 32 * KB
    BPT = [Buf("pt%d" % i) for i in range(4)]
    BRD = [Buf("rd0"), Buf("rd1")]
    PT = [carve(o5_ + i * KB, [512], BF16, BPT[i]) for i in range(4)]
    RD = [carve(o5_ + 4 * KB + i * 2 * KB, [512], F32, BRD[i]) for i in range(2)]
    SCALE = 128.0 ** -0.5
    it = 0
    pend_a = mod_cols(3 * D, 2 * D, R3 + 40 * KB, 7, 64, False, None, defer=True)

    def mod2_finish():
        P.op("dve", I("tensor_tensor", out=modB, in0=ps(7)[:, 64:128].rearrange("p (a b) -> p a b", b=2),
                      in1=modbT[:, 48:80].unsqueeze(2).broadcast_to([128, 32, 2]), op=ALU.add), reads=[PSB[7], Bc], writes=[Bmod])
        P.op("dve", I("tensor_scalar", out=S2, in0=modB[:, 16:32, 0], scalar1=1.0, scalar2=None, op0=ALU.add), reads=[Bmod], writes=[Bmod])
        P.op("dve", I("tensor_tensor", out=S2, in0=S2, in1=n2T, op=ALU.mult), reads=[Bmod, Bc], writes=[Bmod])
        P.op("dve", I("tensor_copy", out=B2, in_=modB[:, 0:16, 0]), reads=[Bmod], writes=[Bmod])

    for h in range(16):
        kvh = h // 4
        for qb in range(2):
            par = it % 2
            it += 1
            if pend_a:
                pend_a.pop(0)()
                if not pend_a:
                    mod2_finish()
            bo, bd = 3 + par, 5 + par
            qbufs = [BQT[h // 4][4 * qb + j] for j in range(4)]
            qrhs = QT[:, h, qb * 512:(qb + 1) * 512]

            def emit_s(kt):
                bs = kt % 3
                P.op("pe", I("matmul", ps(bs), lhsT=KT[:, kvh, kt * 128:(kt + 1) * 128], rhs=qrhs, start=True, stop=True),
                     reads=[BKT[kt]] + qbufs, writes=[PSB[bs]])

            emit_s(0)
            emit_s(1)
            for kt in range(34):
                if kt + 2 < 34:
                    emit_s(kt + 2)
                bs = kt % 3
                pi = kt % 4
                P.op("act", I("activation", out=PT[pi], in_=ps(bs), func=AF.Exp, scale=SCALE), reads=[PSB[bs]], writes=[BPT[pi]])
                fns = [I("matmul", ps(bo), lhsT=VS[:, kt, kvh * 128:(kvh + 1) * 128], rhs=PT[pi], start=(kt == 0), stop=(kt == 33)),
                       I("matmul", ps(bd), lhsT=onesb, rhs=PT[pi], start=(kt == 0), stop=(kt == 33))]
                P.op("pe", fns, reads=[BVS[kt], BPT[pi], Bc], writes=[PSB[bo], PSB[bd]])
            P.op("dve", I("reciprocal", out=RD[par], in_=ps(bd)), reads=[PSB[bd]], writes=[BRD[par]])
            P.op("dve", I("tensor_tensor", out=YAT[:, h, qb * 512:(qb + 1) * 512], in0=ps(bo), in1=RD[par], op=ALU.mult),
                 reads=[PSB[bo], BRD[par]], writes=[BYA[h][qb]])
    dump("d_YAT", YAT.rearrange("p a b -> p (a b)"), [128, 16 * 1024], BF16, flat(BYA))
    if stop_after <= 5:
        return finish()

    BHTO = [[Buf("hto2_%d_%d" % (t, g)) for g in range(4)] for t in range(8)]
    HTO = carve(R2, [16, NOWN], BF16, flat(BHTO))
    BYT = [[Buf("yt%d_%d" % (dc, tb)) for tb in range(2)] for dc in range(16)]
    YT = carve(R2 + 32 * KB, [16, NOWN], BF16, flat(BYT))
    o6 = R3 + 56 * KB
    BXS6, BXN6 = [Buf("xs6"), Buf("xs6b")], [Buf("xn6"), Buf("xn6b")]
    XS6 = [carve(o6, [D], F32, BXS6[0]), carve(R3 + 28 * KB, [D], F32, BXS6[1])]
    XN6 = [carve(o6 + 8 * KB, [D], BF16, BXN6[0]), carve(R3 + 36 * KB, [D], BF16, BXN6[1])]

    def s6_A(t):
        i = t % 2
        nt_compute(0, XS6[i], XN6[i], i, (lambda t: lambda c: HTO[:, c, t * 128:(t + 1) * 128])(t), BHTO[t], BXS6[i], BXN6[i], bank0=2 * i)

    for t in range(0, 8, 2):
        for i in range(2):
            nt_load(xall[(t + i) * 128:(t + i + 1) * 128, :], XS6[i], BXS6[i])
        P.play([P.rec(s6_A, t), P.rec(s6_A, t + 1)])
    BWS = [Buf("ws0"), Buf("ws1")]
    WS = [carve(R3 + i * 28 * KB, [56, 256], BF16, BWS[i]) for i in range(2)]
    BSG = [Buf("sg%d" % i) for i in range(4)]
    SG = [carve(o6 + 12 * KB + i * 2 * KB, [512], F32, BSG[i]) for i in range(4)]
    step = 0
    def ws_load(cb):
        s = cb % 2
        c0 = cb * 256
        srcs = [(w_fo[:, c0:c0 + 256], 0, 8), (w_ao[:, c0:c0 + 256], 8, 16),
                (w_in[:, GF_OFF + c0:GF_OFF + c0 + 256], 24, 16), (w_in[:, GA_OFF + c0:GA_OFF + c0 + 256], 40, 16)]
        for (srcd, k0, nk) in srcs:
            src = srcd.rearrange("(c p) f -> p c f", p=128)
            P.dma("pool", I("dma_start", out=WS[s][:, k0:k0 + nk, :], in_=src), writes=[BWS[s]], sem="ws%d" % s)

    ws_load(0)
    for cb in range(8):
        s = cb % 2
        if cb + 1 < 8:
            ws_load(cb + 1)
        for cc in range(2):
            dc = 2 * cb + cc
            for tb in range(2):
                par = step % 2
                step += 1
                b_fo, b_ao, b_gf, b_ga = 4 * par, 4 * par + 1, 4 * par + 2, 4 * par + 3
                tsl = slice(tb * 512, (tb + 1) * 512)
                csl = slice(cc * 128, (cc + 1) * 128)
                fns = [I("matmul", ps(b_fo), lhsT=WS[s][:, k, csl], rhs=YFT[:, k, tsl], start=(k == 0), stop=(k == 7)) for k in range(8)]
                P.op("pe", fns, reads=[BWS[s]] + [BYF[g][tb] for g in range(8)], writes=[PSB[b_fo]])
                fns = [I("matmul", ps(b_ao), lhsT=WS[s][:, 8 + k, csl], rhs=YAT[:, k, tsl], start=(k == 0), stop=(k == 15)) for k in range(16)]
                P.op("pe", fns, reads=[BWS[s]] + [BYA[h][tb] for h in range(16)], writes=[PSB[b_ao]])
                hbufs = [b for t in range(4 * tb, 4 * tb + 4) for b in BHTO[t]]
                fns = [I("matmul", ps(b_gf), lhsT=WS[s][:, 24 + k, csl], rhs=HTO[:, k, tsl], start=(k == 0), stop=(k == 15)) for k in range(16)]
                P.op("pe", fns, reads=[BWS[s]] + hbufs, writes=[PSB[b_gf]])
                fns = [I("matmul", ps(b_ga), lhsT=WS[s][:, 40 + k, csl], rhs=HTO[:, k, tsl], start=(k == 0), stop=(k == 15)) for k in range(16)]
                P.op("pe", fns, reads=[BWS[s]] + hbufs, writes=[PSB[b_ga]])
                P.op("act", I("activation", out=SG[0], in_=ps(b_gf), func=AF.Sigmoid), reads=[PSB[b_gf]], writes=[BSG[0]])
                P.op("act", I("activation", out=SG[1], in_=ps(b_ga), func=AF.Sigmoid), reads=[PSB[b_ga]], writes=[BSG[1]])
                P.op("dve", I("tensor_tensor", out=SG[2], in0=ps(b_fo), in1=SG[0], op=ALU.mult), reads=[PSB[b_fo], BSG[0]], writes=[BSG[2]])
                P.op("dve", I("tensor_tensor", out=SG[3], in0=ps(b_ao), in1=SG[1], op=ALU.mult), reads=[PSB[b_ao], BSG[1]], writes=[BSG[3]])
                P.op("pool", I("tensor_tensor", out=YT[:, dc, tsl], in0=SG[2], in1=SG[3], op=ALU.add), reads=[BSG[2], BSG[3]], writes=[BYT[dc][tb]])
    dump("d_YT", YT.rearrange("p a b -> p (a b)"), [128, 16 * 1024], BF16, flat(BYT))
    if stop_after <= 6:
        return finish()

    BG1 = Buf("g1")
    G1 = carve(12 * KB, [D], F32, BG1)
    P.dma("sp", I("dma_start", out=G1, in_=mod_b[0:1, 2 * D:3 * D].partition_broadcast(128)), writes=[BG1], sem="g1")

    def evac_g1(j, bk):
        P.op("dve", I("tensor_tensor", out=G1[:, j * 256:(j + 1) * 256], in0=ps(bk)[:, 0:256], in1=G1[:, j * 256:(j + 1) * 256], op=ALU.add),
             reads=[PSB[bk]], writes=[BG1])

    mod_cols(2 * D, D, R1, 0, 0, True, evac_g1)
    dump("d_G1", G1, [128, D], F32, [BG1])

    BX1 = [[Buf("x1_%d_%d" % (t, db)) for db in range(4)] for t in range(8)]
    X1 = carve(R3, [8, D], F32, flat(BX1))
    for t in range(8):
        P.dma("sp", I("dma_start", out=X1[:, t, :], in_=xall[t * 128:(t + 1) * 128, :]), writes=BX1[t], sem="x1_%d" % t)
    BWO = [Buf("wo0"), Buf("wo1")]
    WO = [carve(R1 + 16 * KB + i * 16 * KB, [16, 512], BF16, BWO[i]) for i in range(2)]
    BTMP = [Buf("tmp0"), Buf("tmp1")]
    TMP = [carve(R2 + 16 * KB + i * 2 * KB, [512], F32, BTMP[i]) for i in range(2)]
    def wo_load(db):
        src = w_o[:, db * 512:(db + 1) * 512].rearrange("(c p) f -> p c f", p=128)
        P.dma("pool", I("dma_start", out=WO[db % 2], in_=src), writes=[BWO[db % 2]], sem="wo%d" % (db % 2))

    BG2F, BG2 = Buf("g2f"), Buf("g2")
    G2F = carve(36 * KB, [D], F32, BG2F)
    G2 = carve(124 * KB, [D], BF16, BG2)
    P.dma("sp", I("dma_start", out=G2F, in_=mod_b[0:1, 5 * D:6 * D].partition_broadcast(128)), writes=[BG2F], sem="g2f")

    def evac_g2(j, bk):
        P.op("dve", I("tensor_tensor", out=G2[:, j * 256:(j + 1) * 256], in0=ps(bk)[:, 0:256], in1=G2F[:, j * 256:(j + 1) * 256], op=ALU.add),
             reads=[PSB[bk], BG2F], writes=[BG2])

    pending = mod_cols(5 * D, D, 20 * KB, 0, 0, True, evac_g2, defer=True)
    wo_load(0)
    for db in range(4):
        s = db % 2
        if db + 1 < 4:
            wo_load(db + 1)
        for t in range(8):
            if pending:
                pending.pop(0)()
            bk = 2 + (t % 4)
            i = t % 2
            fns = [I("matmul", ps(bk), lhsT=YT[:, k, t * 128:(t + 1) * 128], rhs=WO[s][:, k, :], start=(k == 0), stop=(k == 15)) for k in range(16)]
            P.op("pe", fns, reads=[BYT[k][t // 4] for k in range(16)] + [BWO[s]], writes=[PSB[bk]])
            P.op("dve", I("tensor_tensor", out=TMP[i], in0=ps(bk), in1=G1[:, db * 512:(db + 1) * 512], op=ALU.mult),
                 reads=[PSB[bk], BG1], writes=[BTMP[i]])
            xv = X1[:, t, db * 512:(db + 1) * 512]
            P.op("dve", I("tensor_tensor", out=xv, in0=xv, in1=TMP[i], op=ALU.add), reads=[BTMP[i]], writes=[BX1[t][db]])
    if debug:
        d = nc.dram_tensor("d_X1", [NOWN, D], F32, kind="ExternalOutput").ap()
        b = Buf("dbg_x1")
        for t in range(8):
            P.dma("sp", I("dma_start", out=d[t * 128:(t + 1) * 128, :], in_=X1[:, t, :]), reads=BX1[t], writes=[b], sem="dbg")
        DBGB.append(b)
    if stop_after <= 7:
        return finish()

    while pending:
        pending.pop(0)()
    dump("d_G2", G2, [128, D], BF16, [BG2])
    dump("d_S2", S2, [128, 16], F32, [Bmod])
    if stop_after <= 7.5:
        return finish()

    BH2 = [[Buf("h2_%d_%d" % (t, g)) for g in range(4)] for t in range(8)]
    H2T = carve(R1, [16, NOWN], BF16, flat(BH2))
    o8 = R2
    BRW, BJ, BSC, BRT, BSM = Buf("rw"), Buf("junk"), Buf("sc"), Buf("rt"), Buf("sm")
    BXN2 = [Buf("xn2_0"), Buf("xn2_1")]
    BH2F = [[Buf("h2f%d_%d" % (i, g)) for g in range(4)] for i in range(2)]
    RW = carve(o8, [16, NE], F32, BRW)
    XN2 = [carve(o8 + 4 * KB + i * 8 * KB, [D], F32, BXN2[i]) for i in range(2)]
    H2F = [carve(o8 + 20 * KB + i * 8 * KB, [16, 128], F32, BH2F[i]) for i in range(2)]
    JUNK = carve(o8 + 36 * KB, [D], BF16, BJ)
    SC = carve(o8 + 40 * KB, [8, NE], F32, BSC)
    RT = [carve(o8 + 42 * KB + i * 2 * KB, [8, NE], F32, BRT) for i in range(4)]
    SM = carve(o8 + 50 * KB, [256], F32, BSM)
    P.dma("sp", I("dma_start", out=RW, in_=router_w.rearrange("(c p) f -> p c f", p=128)), writes=[BRW], sem="rw")

    def s8_A(t):
        s = t % 4
        stt = stat[s]
        xn2 = XN2[t % 2]
        P.op("pool", I("memset", stt[:, 0:1], 0.0), writes=[Bstat[s]])
        P.op("act", I("activation", out=JUNK, in_=X1[:, t, :], func=AF.Square, accum_out=stt[:, 0:1]), reads=BX1[t], writes=[BJ, Bstat[s]])
        P.op("act", I("activation", out=stt[:, 1:2], in_=stt[:, 0:1], func=AF.Sqrt, scale=1.0 / D, bias=EPS), reads=[Bstat[s]], writes=[Bstat[s]])
        P.op("dve", I("reciprocal", out=stt[:, 2:3], in_=stt[:, 1:2]), reads=[Bstat[s]], writes=[Bstat[s]])
        P.op("dve", I("tensor_scalar", out=xn2, in0=X1[:, t, :], scalar1=stt[:, 2:3], scalar2=None, op0=ALU.mult), reads=BX1[t] + [Bstat[s]], writes=[BXN2[t % 2]])

    def s8_B(t):
        xn2, h2f = XN2[t % 2], H2F[t % 2]
        for g in range(4):
            bk = g
            fns = [I("transpose", ps(bk)[:, j * 128:(j + 1) * 128], xn2[:, (4 * g + j) * 128:(4 * g + j + 1) * 128], identf) for j in range(4)]
            P.op("pe", fns, reads=[BXN2[t % 2], Bc], writes=[PSB[bk]])
            fns = [I("activation", out=H2T[:, 4 * g + j, t * 128:(t + 1) * 128], in_=ps(bk)[:, j * 128:(j + 1) * 128], func=AF.Identity,
                     scale=S2[:, 4 * g + j:4 * g + j + 1], bias=B2[:, 4 * g + j:4 * g + j + 1]) for j in range(4)]
            P.op("act", fns, reads=[PSB[bk], Bmod], writes=[BH2[t][g]])
            fns = [I("tensor_scalar", out=h2f[:, 4 * g + j, :], in0=ps(bk)[:, j * 128:(j + 1) * 128], scalar1=S2[:, 4 * g + j:4 * g + j + 1],
                     scalar2=B2[:, 4 * g + j:4 * g + j + 1], op0=ALU.mult, op1=ALU.add) for j in range(4)]
            P.op("dve", fns, reads=[PSB[bk], Bmod], writes=[BH2F[t % 2][g]])

    def s8_C(t):
        h2f = H2F[t % 2]
        br = 4 + (t % 2)
        fns = [I("matmul", ps(br)[:, 0:NE], lhsT=h2f[:, c, :], rhs=RW[:, c, :], start=(c == 0), stop=(c == 15)) for c in range(16)]
        P.op("pe", fns, reads=BH2F[t % 2] + [BRW], writes=[PSB[br]])

    def s8_Cev(t):
        br = 4 + (t % 2)
        P.op("act", I("activation", out=SC[:, t, :], in_=ps(br)[:, 0:NE], func=AF.Sigmoid), reads=[PSB[br]], writes=[BSC])

    for i in range(10):
        ch = []
        if i < 8:
            ch.append(P.rec(s8_A, i))
        if 0 <= i - 1 < 8:
            ch.append(P.rec(s8_B, i - 1))
        if 0 <= i - 2 < 8:
            ch.insert(0, P.rec(s8_C, i - 2))
        P.play(ch)
        if 0 <= i - 2 < 8:
            s8_Cev(i - 2)
    if stop_after <= 7.7:
        dump("d_H2T", H2T.rearrange("p a b -> p (a b)"), [128, 16 * 1024], BF16, flat(BH2))
        dump("d_SC", SC.rearrange("p a b -> p (a b)"), [128, 8 * NE], F32, [BSC])
        return finish()
    BI, M1, GS, PEN = RT[0], SM[:, 0:64], SM[:, 64:128], SM[:, 128:192]
    TOP = SM[:, 192:200]
    THR = SM[:, 200:216]
    WS_ = SM[:, 216:232]
    g8 = lambda ap: ap.rearrange("p t (g k) -> p (t g) k", k=8)
    bi4, e4 = g8(BI), g8(RT[1])
    P.op("dve", I("tensor_tensor", out=BI, in0=SC, in1=rb_bc.unsqueeze(1).broadcast_to([128, 8, NE]), op=ALU.add), reads=[BSC, Bc], writes=[BRT])
    P.op("dve", I("tensor_reduce", out=M1, in_=bi4, axis=AX.X, op=ALU.max), reads=[BRT], writes=[BSM])
    P.op("dve", I("tensor_tensor", out=e4, in0=bi4, in1=M1.unsqueeze(2).broadcast_to([128, 64, 8]), op=ALU.is_equal), reads=[BRT, BSM], writes=[BRT])
    P.op("dve", I("scalar_tensor_tensor", out=e4, in0=e4, scalar=-1e30, in1=bi4, op0=ALU.mult, op1=ALU.add), reads=[BRT], writes=[BRT])
    P.op("dve", I("tensor_reduce", out=GS, in_=e4, axis=AX.X, op=ALU.max), reads=[BRT], writes=[BSM])
    P.op("dve", I("tensor_tensor", out=GS, in0=GS, in1=M1, op=ALU.add), reads=[BSM], writes=[BSM])
    for t in range(8):
        P.op("dve", I("max", out=TOP, in_=GS[:, t * 8:(t + 1) * 8]), reads=[BSM], writes=[BSM])
        P.op("dve", I("tensor_copy", out=THR[:, t:t + 1], in_=TOP[:, 3:4]), reads=[BSM], writes=[BSM])
    gs3 = GS.rearrange("p (t g) -> p t g", g=8)
    pen3 = PEN.rearrange("p (t g) -> p t g", g=8)
    P.op("dve", I("tensor_tensor", out=pen3, in0=gs3, in1=THR[:, 0:8].unsqueeze(2).broadcast_to([128, 8, 8]), op=ALU.is_ge), reads=[BSM], writes=[BSM])
    P.op("dve", I("tensor_scalar", out=PEN, in0=PEN, scalar1=1e30, scalar2=-1e30, op0=ALU.mult, op1=ALU.add), reads=[BSM], writes=[BSM])
    MK = RT[2]
    P.op("dve", I("tensor_tensor", out=g8(MK), in0=bi4, in1=PEN.unsqueeze(2).broadcast_to([128, 64, 8]), op=ALU.add), reads=[BRT, BSM], writes=[BRT])
    for t in range(8):
        P.op("dve", I("max", out=TOP, in_=MK[:, t, :]), reads=[BRT, BSM], writes=[BSM])
        P.op("dve", I("tensor_copy", out=THR[:, 8 + t:9 + t], in_=TOP[:, 7:8]), reads=[BSM], writes=[BSM])
    SEL = RT[3]
    P.op("dve", I("tensor_tensor", out=SEL, in0=MK, in1=THR[:, 8:16].unsqueeze(2).broadcast_to([128, 8, NE]), op=ALU.is_ge), reads=[BRT, BSM], writes=[BRT])
    P.op("dve", I("tensor_tensor", out=SEL, in0=SEL, in1=SC, op=ALU.mult), reads=[BRT, BSC], writes=[BRT])
    P.op("dve", I("tensor_reduce", out=WS_[:, 0:8], in_=SEL, axis=AX.X, op=ALU.add), reads=[BRT], writes=[BSM])
    P.op("dve", I("reciprocal", out=WS_[:, 8:16], in_=WS_[:, 0:8]), reads=[BSM], writes=[BSM])
    P.op("dve", I("tensor_tensor", out=wdense, in0=SEL, in1=WS_[:, 8:16].unsqueeze(2).broadcast_to([128, 8, NE]), op=ALU.mult), reads=[BRT, BSM], writes=[Bwd])
    P.op("dve", I("tensor_scalar", out=wdense, in0=wdense, scalar1=2.5, scalar2=None, op0=ALU.mult), reads=[Bwd], writes=[Bwd])
    dump("d_H2T", H2T.rearrange("p a b -> p (a b)"), [128, 16 * 1024], BF16, flat(BH2))
    dump("d_WD", wdense.rearrange("p a b -> p (a b)"), [128, 8 * NE], F32, [Bwd])
    dump("d_SC", SC.rearrange("p a b -> p (a b)"), [128, 8 * NE], F32, [BSC])
    if stop_after <= 8:
        return finish()

    BRING = [Buf("ring%d" % i) for i in range(4)]
    RING = [carve(R2 + i * 16 * KB, [8192], BF16, BRING[i]) for i in range(4)]
    BAT = [[[Buf("at%d_%d_%d" % (p, fc, tb)) for tb in range(2)] for fc in range(4)] for p in range(2)]
    AT = [carve(192 * KB, [4, NOWN], BF16, flat(BAT[0])), carve(16 * KB, [4, NOWN], BF16, flat(BAT[1]))]
    NEXP = NE + 1

    def wsrc(e_, m):
        if e_ < NE:
            return (exp_gate[e_], exp_up[e_], exp_down[e_])[m]
        return (sh_gate, sh_up, sh_down)[m]

    blocks = [(e_, m) for e_ in range(NEXP) for m in range(3)]

    def issue(k):
        e_, m = blocks[k]
        s = k % 4
        nchunk = 16 if m < 2 else 4
        dst = RING[s].rearrange("p (c f) -> p c f", c=nchunk)
        src = wsrc(e_, m).rearrange("(c p) f -> p c f", p=128)
        P.dma("pool", I("dma_start", out=dst, in_=src), writes=[BRING[s]], sem="ring%d" % s)
        if m == 2:
            P.op("pool", I("tensor_tensor", out=dst, in0=dst, in1=G2.unsqueeze(1).broadcast_to([128, 4, D]), op=ALU.mult),
                 reads=[BG2], writes=[BRING[s]])

    for k in range(3):
        issue(k)
    for e_ in range(NEXP):
        par = e_ % 2
        kg, ku, kd = 3 * e_, 3 * e_ + 1, 3 * e_ + 2
        if kg + 3 < len(blocks):
            issue(kg + 3)
        for (kk, is_gate) in ((kg, True), (ku, False)):
            if not is_gate and ku + 3 < len(blocks):
                issue(ku + 3)
            W = RING[kk % 4].rearrange("p (c f) -> p c f", c=16)
            for fc in range(4):
                for tb in range(2):
                    bk = (fc * 2 + tb) % 4
                    fns = [I("matmul", ps(bk), lhsT=W[:, c, fc * 128:(fc + 1) * 128], rhs=H2T[:, c, tb * 512:(tb + 1) * 512],
                             start=(c == 0), stop=(c == 15)) for c in range(16)]
                    P.op("pe", fns, reads=[BRING[kk % 4]] + [b for t in range(4 * tb, 4 * tb + 4) for b in BH2[t]], writes=[PSB[bk]])
                    av = AT[par][:, fc, tb * 512:(tb + 1) * 512]
                    if is_gate:
                        P.op("act", I("activation", out=av, in_=ps(bk), func=AF.Silu), reads=[PSB[bk]], writes=[BAT[par][fc][tb]])
                    else:
                        P.op("dve", I("tensor_tensor", out=av, in0=ps(bk), in1=av, op=ALU.mult), reads=[PSB[bk], BAT[par][fc][tb]], writes=[BAT[par][fc][tb]])
        if kd + 3 < len(blocks):
            issue(kd + 3)
        WD = RING[kd % 4].rearrange("p (c f) -> p c f", c=4)
        for t in range(8):
            for db in range(4):
                bk = 4 + db
                fns = [I("matmul", ps(bk), lhsT=AT[par][:, fc, t * 128:(t + 1) * 128], rhs=WD[:, fc, db * 512:(db + 1) * 512],
                         start=(fc == 0), stop=(fc == 3)) for fc in range(4)]
                P.op("pe", fns, reads=[BRING[kd % 4]] + [BAT[par][fc][t // 4] for fc in range(4)], writes=[PSB[bk]])
                xv = X1[:, t, db * 512:(db + 1) * 512]
                if e_ < NE:
                    P.op("dve", I("scalar_tensor_tensor", out=xv, in0=ps(bk), scalar=wdense[:, t, e_:e_ + 1], in1=xv, op0=ALU.mult, op1=ALU.add),
                         reads=[PSB[bk], Bwd], writes=[BX1[t][db]])
                else:
                    P.op("dve", I("tensor_tensor", out=xv, in0=ps(bk), in1=xv, op=ALU.add), reads=[PSB[bk]], writes=[BX1[t][db]])
    for t in range(8):
        b = Buf("out%d" % t)
        P.dma("sp", I("dma_start", out=yout[t * 128:(t + 1) * 128, :], in_=X1[:, t, :]), reads=BX1[t], writes=[b], sem="out")
        OUTB.append(b)
    return finish()


_CACHE = {}


def _const_tables():
    if "tabs" in _CACHE:
        return _CACHE["tabs"]
    bf = ml_dtypes.bfloat16
    n = np.arange(S, dtype=np.int64)
    tabs = {}
    tabs["identb"] = np.eye(128, dtype=np.float32).astype(bf)
    tabs["identf"] = np.eye(128, dtype=np.float32)
    tabs["onesb"] = np.ones((128, 128), dtype=np.float32).astype(bf)
    c = np.arange(128, dtype=np.int64)
    ang = 2.0 * np.pi * ((c[:, None] * c[None, :]) % 128) / 128.0
    sc = 1.0 / np.sqrt(float(S) * 128.0)
    tabs["chC"] = (np.cos(ang) * sc).astype(np.float32).astype(bf)
    tabs["chS"] = (-np.sin(ang) * sc).astype(np.float32).astype(bf)
    inv = (10000.0 ** (-np.arange(32, dtype=np.float32) / 32.0)).astype(np.float32)
    pos = np.stack([n // 64, n % 64], axis=-1).astype(np.float32)
    a = (pos[:, :, None] * inv[None, None, :]).astype(np.float32)
    tabs["cos"] = np.cos(a).astype(np.float32).reshape(S, 64)
    tabs["sin"] = np.sin(a).astype(np.float32).reshape(S, 64)
    _CACHE["tabs"] = tabs
    return tabs


def _core_order(j):
    own = np.arange(j * NOWN, (j + 1) * NOWN)
    rest = np.concatenate([np.arange(0, j * NOWN), np.arange((j + 1) * NOWN, S)])
    return np.concatenate([own, rest]).astype(np.int64)


def prepare_inputs(inp):
    bf = ml_dtypes.bfloat16
    tabs = _const_tables()
    f32 = lambda a: np.ascontiguousarray(np.asarray(a, dtype=np.float32))
    x = f32(inp["x"])
    c = f32(inp["c"])
    ctx = f32(inp["ctx"])
    c_ctx = f32(inp["c_ctx"])
    shared = {
        "mod_w": f32(inp["mod_w"][0]),
        "mod_bT": f32(inp["mod_b"][0].reshape(96, 128).T),
        "mod_b": f32(inp["mod_b"][0].reshape(1, -1)),
        "norm1T": f32(inp["norm1_w"][0].reshape(16, 128).T),
        "norm2T": f32(inp["norm2_w"][0].reshape(16, 128).T),
        "w_in": f32(inp["w_in"][0]),
        "qnw": f32(inp["q_norm_w"][0].reshape(1, 128)),
        "knw": f32(inp["k_norm_w"][0].reshape(1, 128)),
        "w_fo": f32(inp["w_fourier_out"][0]),
        "w_ao": f32(inp["w_attn_out"][0]),
        "w_o": f32(inp["w_out"][0]),
        "router_w": f32(inp["router_w"][0]),
        "router_b": f32(inp["router_b"][0].reshape(1, NE)),
        "exp_gate": f32(inp["exp_gate"][0]),
        "exp_up": f32(inp["exp_up"][0]),
        "exp_down": f32(inp["exp_down"][0]),
        "sh_gate": f32(inp["shared_gate"][0]),
        "sh_up": f32(inp["shared_up"][0]),
        "sh_down": f32(inp["shared_down"][0]),
        "identb": tabs["identb"], "identf": tabs["identf"], "onesb": tabs["onesb"],
        "chC": tabs["chC"], "chS": tabs["chS"],
    }
    dft = {}
    for j in range(4):
        order = _core_order(j)
        k = np.arange(j * NOWN, (j + 1) * NOWN, dtype=np.int64)
        ang = (2.0 * np.pi / S) * ((order[:, None] * k[None, :]) % S).astype(np.float64)
        dft[j] = (np.cos(ang).astype(np.float32).astype(bf), np.sin(ang).astype(np.float32).astype(bf),
                  np.ascontiguousarray(tabs["cos"][order]), np.ascontiguousarray(tabs["sin"][order]), order)
    in_maps = []
    for core in range(8):
        b, j = core // 4, core % 4
        dC, dS, cs, sn, order = dft[j]
        cond = np.stack([c[b], c_ctx], axis=-1)
        condT = cond.reshape(16, 128, 2).transpose(1, 0, 2).reshape(128, 32)
        m = dict(shared)
        m.update({
            "xall": np.ascontiguousarray(x[b][order]),
            "ctxb": np.ascontiguousarray(ctx[b]),
            "condT": np.ascontiguousarray(condT),
            "cosA": cs, "sinA": sn, "dftC": dC, "dftS": dS,
        })
        in_maps.append(m)
    return in_maps


def kernel(**inputs):
    if "nc" not in _CACHE:
        _CACHE["nc"] = build_program()
    nc = _CACHE["nc"]
    in_maps = prepare_inputs(inputs)
    res = run_bass_kernel_spmd(nc, in_maps, core_ids=list(range(8)))
    out = np.empty((2, S, D), dtype=np.float32)
    for core in range(8):
        b, j = core // 4, core % 4
        out[b, j * NOWN:(j + 1) * NOWN, :] = res.results[core]["yout"]
    return out
```

```python
from contextlib import ExitStack
import numpy as np
import ml_dtypes
import concourse.bass as bass
import concourse.mybir as mybir
from concourse.bass_utils import run_bass_kernel_spmd

F32 = mybir.dt.float32
BF16 = mybir.dt.bfloat16
U8 = mybir.dt.uint8
AF = mybir.ActivationFunctionType
ALU = mybir.AluOpType
AX = mybir.AxisListType

D = 2048
S = 4096
NOWN = 1024
NCTX = 256
NE = 64
EPS = 1e-6
Q_OFF, K_OFF, V_OFF, GF_OFF, GA_OFF = 1024, 3072, 3584, 4096, 6144


class Buf:
    __slots__ = ("name", "lw", "rd", "excl")

    def __init__(self, name="", excl=False):
        self.name = name
        self.lw = None
        self.rd = {}
        self.excl = excl


class Prog:
    ENGS = ("pe", "act", "dve", "pool", "sp")

    def __init__(self, nc):
        self.nc = nc
        self.lists = {e: [] for e in self.ENGS}
        self.cnt = {e: 0 for e in self.ENGS}
        self.waited = {e: {} for e in self.ENGS}
        self.dma_sems = {}
        self.sem_handles = {}
        self._rec = None

    def rec(self, fn, *a, **k):
        self._rec = []
        fn(*a, **k)
        r, self._rec = self._rec, None
        return r

    def play(self, lists):
        idx = [0] * len(lists)
        left = sum(len(l) for l in lists)
        while left:
            for j, l in enumerate(lists):
                if idx[j] < len(l):
                    kind, a, k = l[idx[j]]
                    idx[j] += 1
                    left -= 1
                    getattr(self, kind)(*a, **k)

    def _deps(self, reads, writes):
        deps = []
        for b in reads:
            if b.lw is not None:
                deps.append(b.lw)
        for b in writes:
            if b.lw is not None:
                deps.append(b.lw)
            deps.extend(b.rd.items())
        return deps

    def _emit_waits(self, eng, deps):
        need = {}
        w = self.waited[eng]
        for (k, v) in deps:
            if v > w.get(k, 0) and v > need.get(k, 0):
                need[k] = v
        for k, v in need.items():
            w[k] = v
            self.lists[eng].append(("wait", k, v))

    def _commit(self, tok, reads, writes):
        k, v = tok
        for b in reads:
            if b.rd.get(k, 0) < v:
                b.rd[k] = v
        for b in writes:
            b.lw = tok
            b.rd = {}

    def op(self, eng, fns, reads=(), writes=()):
        if self._rec is not None:
            self._rec.append(("op", (eng, fns, list(reads), list(writes)), {}))
            return None
        if not isinstance(fns, (list, tuple)):
            fns = [fns]
        ex = [b for b in reads if b.excl]
        if ex:
            writes = list(writes) + ex
        self._emit_waits(eng, self._deps(reads, writes))
        self.cnt[eng] += 1
        tok = ("e_" + eng, self.cnt[eng])
        L = self.lists[eng]
        for f in fns[:-1]:
            L.append(("ins", f, None))
        L.append(("ins", fns[-1], ("e_" + eng, 1)))
        self._commit(tok, reads, writes)
        return tok

    def dma(self, eng, fn, reads=(), writes=(), sem=None):
        if self._rec is not None:
            self._rec.append(("dma", (eng, fn, list(reads), list(writes), sem), {}))
            return None
        self._emit_waits(eng, self._deps(reads, writes))
        sem = "d_" + sem
        self.dma_sems[sem] = self.dma_sems.get(sem, 0) + 16
        tok = (sem, self.dma_sems[sem])
        self.lists[eng].append(("ins", fn, (sem, 16)))
        self._commit(tok, reads, writes)
        return tok

    def wait_all(self, eng, bufs):
        deps = [b.lw for b in bufs if b.lw is not None]
        self._emit_waits(eng, deps)

    def emit(self, stack):
        nc = self.nc
        keys = ["e_" + e for e in self.ENGS] + sorted(self.dma_sems.keys())
        for k in keys:
            self.sem_handles[k] = stack.enter_context(nc.semaphore(k))
        block = stack.enter_context(nc.Block())
        H = self.sem_handles

        def run(e, items):
            for it in items:
                if it[0] == "wait":
                    e.wait_ge(H[it[1]], it[2])
                else:
                    ins = it[1](e)
                    if it[2] is not None:
                        ins.then_inc(H[it[2][0]], it[2][1])

        @block.tensor
        def _(e):
            run(e, self.lists["pe"])

        @block.scalar
        def _(e):
            run(e, self.lists["act"])

        @block.vector
        def _(e):
            run(e, self.lists["dve"])

        @block.gpsimd
        def _(e):
            run(e, self.lists["pool"])

        @block.sync
        def _(e):
            run(e, self.lists["sp"])


def I(method, *args, **kw):
    return lambda e: getattr(e, method)(*args, **kw)


def build_program(debug=False, stop_after=99):
    nc = bass.Bass("TRN2", target_bir_lowering=False)

    def din(name, shape, dt=F32):
        return nc.dram_tensor(name, list(shape), dt, kind="ExternalInput").ap()

    xall = din("xall", [S, D])
    ctxb = din("ctxb", [NCTX, D])
    condT = din("condT", [128, 32])
    mod_w = din("mod_w", [D, 6 * D])
    mod_bT = din("mod_bT", [128, 96])
    mod_b = din("mod_b", [1, 6 * D])
    norm1T = din("norm1T", [128, 16])
    norm2T = din("norm2T", [128, 16])
    w_in = din("w_in", [D, 8192])
    qnw = din("qnw", [1, 128])
    knw = din("knw", [1, 128])
    w_fo = din("w_fo", [1024, D])
    w_ao = din("w_ao", [D, D])
    w_o = din("w_o", [D, D])
    router_w = din("router_w", [D, NE])
    router_b = din("router_b", [1, NE])
    exp_gate = din("exp_gate", [NE, D, 512])
    exp_up = din("exp_up", [NE, D, 512])
    exp_down = din("exp_down", [NE, 512, D])
    sh_gate = din("sh_gate", [D, 512])
    sh_up = din("sh_up", [D, 512])
    sh_down = din("sh_down", [512, D])
    identb_d = din("identb", [128, 128], BF16)
    identf_d = din("identf", [128, 128])
    onesb_d = din("onesb", [128, 128], BF16)
    chC_d = din("chC", [128, 128], BF16)
    chS_d = din("chS", [128, 128], BF16)
    cosA = din("cosA", [S, 64])
    sinA = din("sinA", [S, 64])
    dftC = din("dftC", [S, NOWN], BF16)
    dftS = din("dftS", [S, NOWN], BF16)
    yout = nc.dram_tensor("yout", [NOWN, D], F32, kind="ExternalOutput").ap()
    dbg = {}

    st = ExitStack()
    KB = 1024
    arena = st.enter_context(nc.sbuf_tensor("arena", [128, 206 * KB], U8))
    psf = [st.enter_context(nc.psum_tensor("ps%d" % i, [128, 512], F32)) for i in range(8)]
    PSB = [Buf("ps%d" % i, excl=True) for i in range(8)]
    P = Prog(nc)

    occupants = []

    def guard(bufs, guards):
        for b in bufs:
            for g in guards:
                if g.lw is not None and b.rd.get(g.lw[0], 0) < g.lw[1]:
                    b.rd[g.lw[0]] = g.lw[1]
                for k, v in g.rd.items():
                    if b.rd.get(k, 0) < v:
                        b.rd[k] = v

    def carve(off, dims, dt, bufs=None):
        n = int(np.prod(dims))
        sz = 4 if dt == F32 else 2
        end = off + n * sz
        assert end <= 206 * KB, (off, dims)
        ap = arena[:, off:end].bitcast(dt)
        if len(dims) == 2:
            ap = ap.rearrange("p (a b) -> p a b", a=dims[0])
        elif len(dims) == 3:
            ap = ap.rearrange("p (a b c) -> p a b c", a=dims[0], b=dims[1])
        if bufs is not None:
            if isinstance(bufs, Buf):
                bufs = [bufs]
            bufs = list(bufs)
            keep = []
            for (o2, e2, b2) in occupants:
                if o2 < end and off < e2:
                    if b2 not in bufs:
                        guard(bufs, [b2])
                    if not (off <= o2 and e2 <= end):
                        keep.append((o2, e2, b2))
                else:
                    keep.append((o2, e2, b2))
            occupants[:] = keep
            for b in bufs:
                occupants.append((off, end, b))
        return ap

    def ps(i):
        return psf[i][:]

    def psb(i):
        return psf[i][:].bitcast(BF16)

    DBGB = []
    OUTB = []

    def dump(name, ap_sb, shape, dt, bufs):
        if not debug:
            return
        d = nc.dram_tensor(name, list(shape), dt, kind="ExternalOutput").ap()
        dbg[name] = d
        b = Buf("dbg_" + name)
        P.dma("sp", I("dma_start", out=d, in_=ap_sb), reads=bufs, writes=[b], sem="dbg")
        DBGB.append(b)

    def finish():
        P.wait_all("sp", DBGB + OUTB)
        P.emit(st)
        st.close()
        return nc

    def flat(bb):
        return [b for x in bb for b in x]

    Bc = Buf("consts")
    Bmod = Buf("mod")
    Bstat = [Buf("stat%d" % i) for i in range(4)]
    Bwd = Buf("wdense")
    o = [0]

    def cst(dims, dt, buf):
        ap = carve(o[0], dims, dt, buf)
        o[0] += int(np.prod(dims)) * (4 if dt == F32 else 2)
        o[0] = (o[0] + 31) // 32 * 32
        return ap

    identb = cst([128], BF16, Bc)
    identf = cst([128], F32, Bc)
    onesb = cst([128], BF16, Bc)
    chC = cst([128], BF16, Bc)
    chS = cst([128], BF16, Bc)
    qnw_bc = cst([128], F32, Bc)
    knw_bc = cst([128], F32, Bc)
    cond_f = cst([32], F32, Bc)
    modbT = cst([96], F32, Bc)
    n1T = cst([16], F32, Bc)
    n2T = cst([16], F32, Bc)
    rb_bc = cst([NE], F32, Bc)
    scT = cst([16, 2], BF16, Bmod)
    scT_rep = cst([16, 128], BF16, Bmod)
    modA = cst([32, 2], F32, Bmod)
    S1 = cst([16, 2], F32, Bmod)
    modB = cst([32, 2], F32, Bmod)
    S2 = cst([16], F32, Bmod)
    B2 = cst([16], F32, Bmod)
    wdense = cst([8, NE], F32, Bwd)
    stat = [cst([4], F32, Bstat[i]) for i in range(4)]
    assert o[0] <= 12 * KB, o[0]

    YFT_OFF = 12 * KB
    R1 = 28 * KB
    R2 = 60 * KB
    R3 = 128 * KB

    for (dst, src) in ((identb, identb_d[:, :]), (identf, identf_d[:, :]), (onesb, onesb_d[:, :]), (chC, chC_d[:, :]),
                       (chS, chS_d[:, :]), (qnw_bc, qnw[0:1, :].partition_broadcast(128)),
                       (knw_bc, knw[0:1, :].partition_broadcast(128)), (cond_f, condT[:, :]), (modbT, mod_bT[:, :]),
                       (n1T, norm1T[:, :]), (n2T, norm2T[:, :]), (rb_bc, router_b[0:1, :].partition_broadcast(128))):
        P.dma("sp", I("dma_start", out=dst, in_=src), writes=[Bc], sem="c0")

    P.op("act", I("activation", out=scT.rearrange("p a b -> p (a b)"), in_=cond_f, func=AF.Silu), reads=[Bc], writes=[Bmod])
    P.op("dve", I("tensor_copy", out=scT_rep, in_=scT[:, :, 0:1].broadcast_to([128, 16, 128])), reads=[Bmod], writes=[Bmod])

    BMS = [Buf("ms0"), Buf("ms1")]
    mcount = [0]

    def mod_cols(col0, ncols, slot_off, psbank, pscol0, row_form, evac, bw=256, defer=False, dma_reads=()):
        nblk = ncols // bw
        slots = [carve(slot_off + i * bw * 32, [16, bw], BF16, BMS[i]) for i in range(2)]
        def blk(j):
            s = mcount[0] % 2
            mcount[0] += 1
            c0 = col0 + bw * j
            src = mod_w[:, c0:c0 + bw].rearrange("(c p) f -> p c f", p=128)
            P.dma("pool", I("dma_start", out=slots[s], in_=src), reads=list(dma_reads), writes=[BMS[s]], sem="ms%d" % s)
            if not row_form:
                for fc in range(bw // 128):
                    ci = (bw // 128) * j + fc
                    outap = ps(psbank)[:, pscol0 + 2 * ci: pscol0 + 2 * ci + 2]
                    fns = [I("matmul", outap, lhsT=slots[s][:, c, fc * 128:(fc + 1) * 128], rhs=scT[:, c, :],
                             start=(c == 0), stop=(c == 15)) for c in range(16)]
                    P.op("pe", fns, reads=[BMS[s], Bmod], writes=[PSB[psbank]])
            else:
                bk = psbank + (j % 2)
                fns = [I("matmul", ps(bk)[:, 0:256], lhsT=scT_rep[:, c, :], rhs=slots[s][:, c, :],
                         start=(c == 0), stop=(c == 15)) for c in range(16)]
                P.op("pe", fns, reads=[BMS[s], Bmod], writes=[PSB[bk]])
                evac(j, bk)

        thunks = [(lambda j: lambda: blk(j))(j) for j in range(nblk)]
        if defer:
            return thunks
        for th in thunks:
            th()

    mod_cols(0, 4096, R3, 7, 0, False, None, bw=512, dma_reads=[Bc])
    P.op("dve", I("tensor_tensor", out=modA, in0=ps(7)[:, 0:64].rearrange("p (a b) -> p a b", b=2),
                  in1=modbT[:, 0:32].unsqueeze(2).broadcast_to([128, 32, 2]), op=ALU.add), reads=[PSB[7], Bc], writes=[Bmod])
    P.op("dve", I("tensor_scalar", out=S1, in0=modA[:, 16:32, :], scalar1=1.0, scalar2=None, op0=ALU.add), reads=[Bmod], writes=[Bmod])
    P.op("dve", I("tensor_tensor", out=S1, in0=S1, in1=n1T.unsqueeze(2).broadcast_to([128, 16, 2]), op=ALU.mult),
         reads=[Bmod, Bc], writes=[Bmod])

    def nt_load(src_rows, xt, Bx):
        P.dma("sp", I("dma_start", out=xt, in_=src_rows), writes=[Bx], sem=Bx.name)

    def norm_transpose_tile(src_rows, r, xt, xn, si, dst_fn, dst_bufs, Bx, Bxn):
        nt_load(src_rows, xt, Bx)
        nt_compute(r, xt, xn, si, dst_fn, dst_bufs, Bx, Bxn)

    def nt_compute(r, xt, xn, si, dst_fn, dst_bufs, Bx, Bxn, bank0=0):
        stt, Bst = stat[si], Bstat[si]
        P.op("pool", I("memset", stt[:, 0:1], 0.0), writes=[Bst])
        P.op("act", I("activation", out=xn, in_=xt, func=AF.Square, accum_out=stt[:, 0:1]), reads=[Bx], writes=[Bxn, Bst])
        P.op("act", I("activation", out=stt[:, 1:2], in_=stt[:, 0:1], func=AF.Sqrt, scale=1.0 / D, bias=EPS), reads=[Bst], writes=[Bst])
        P.op("dve", I("reciprocal", out=stt[:, 2:3], in_=stt[:, 1:2]), reads=[Bst], writes=[Bst])
        P.op("dve", I("tensor_scalar", out=xn, in0=xt, scalar1=stt[:, 2:3], scalar2=None, op0=ALU.mult), reads=[Bx, Bst], writes=[Bxn])
        for g in range(4):
            bk = bank0 + g % 2
            pv = psb(bk)[:, 0:512]
            fns = [I("transpose", pv[:, j * 128:(j + 1) * 128], xn[:, (4 * g + j) * 128:(4 * g + j + 1) * 128], identb) for j in range(4)]
            P.op("pe", fns, reads=[Bxn, Bc], writes=[PSB[bk]])
            if g % 2 == 0:
                fns = [I("activation", out=dst_fn(4 * g + j), in_=pv[:, j * 128:(j + 1) * 128], func=AF.Identity,
                         scale=S1[:, 4 * g + j, r:r + 1], bias=modA[:, 4 * g + j, r:r + 1]) for j in range(4)]
                P.op("act", fns, reads=[PSB[bk], Bmod], writes=[dst_bufs[g]])
            else:
                fns = [I("tensor_scalar", out=dst_fn(4 * g + j), in0=pv[:, j * 128:(j + 1) * 128], scalar1=S1[:, 4 * g + j, r:r + 1],
                         scalar2=modA[:, 4 * g + j, r:r + 1], op0=ALU.mult, op1=ALU.add) for j in range(4)]
                P.op("dve", fns, reads=[PSB[bk], Bmod], writes=[dst_bufs[g]])

    dump("d_S1", S1.rearrange("p a b -> p (a b)"), [128, 32], F32, [Bmod])
    dump("d_modA", modA.rearrange("p a b -> p (a b)"), [128, 64], F32, [Bmod])
    if stop_after <= 0:
        return finish()

    BWU = [Buf("wu0"), Buf("wu1")]
    WU = carve(R1, [16, 1024], BF16, BWU)
    for h in range(2):
        src = w_in[:, h * 512:(h + 1) * 512].rearrange("(c p) f -> p c f", p=128)
        P.dma("pool", I("dma_start", out=WU[:, :, h * 512:(h + 1) * 512], in_=src), writes=[BWU[h]], sem="wu%d" % h)
    BU = [[Buf("u%d_%d" % (t, n)) for n in range(2)] for t in range(32)]
    U = carve(R2, [32, 1024], BF16, flat(BU))
    BXS = [Buf("xs0"), Buf("xs1")]
    BXN = [Buf("xn0"), Buf("xn1")]
    BHT = [[Buf("ht%d_%d" % (s, g)) for g in range(4)] for s in range(2)]
    XS = [carve(R3 + i * 8 * KB, [D], F32, BXS[i]) for i in range(2)]
    XN = [carve(R3 + 16 * KB + i * 4 * KB, [D], BF16, BXN[i]) for i in range(2)]
    HT = [carve(R3 + 24 * KB + i * 4 * KB, [16, 128], BF16, BHT[i]) for i in range(2)]
    def s1_A(t):
        s = t % 2
        nt_compute(0, XS[s], XN[s], s, (lambda s: lambda c: HT[s][:, c, :])(s), BHT[s], BXS[s], BXN[s])

    def s1_Bmm(t):
        s = t % 2
        for n in range(2):
            bk = 2 + (2 * t + n) % 4
            fns = [I("matmul", ps(bk), lhsT=HT[s][:, c, :], rhs=WU[:, c, n * 512:(n + 1) * 512], start=(c == 0), stop=(c == 15)) for c in range(16)]
            P.op("pe", fns, reads=BHT[s] + [BWU[n]], writes=[PSB[bk]])

    def s1_Bev(t):
        for n in range(2):
            bk = 2 + (2 * t + n) % 4
            if n == 0:
                P.op("act", I("activation", out=U[:, t, n * 512:(n + 1) * 512], in_=ps(bk), func=AF.Identity), reads=[PSB[bk]], writes=[BU[t][n]])
            else:
                P.op("dve", I("tensor_copy", out=U[:, t, n * 512:(n + 1) * 512], in_=ps(bk)), reads=[PSB[bk]], writes=[BU[t][n]])

    nt_load(xall[0:128, :], XS[0], BXS[0])
    for i in range(33):
        if i + 1 < 32:
            nt_load(xall[(i + 1) * 128:(i + 2) * 128, :], XS[(i + 1) % 2], BXS[(i + 1) % 2])
        bm = P.rec(s1_Bmm, i - 1) if i >= 1 else []
        a = P.rec(s1_A, i) if i < 32 else []
        P.play([bm[0:1] + a[0:9] + bm[1:2] + a[9:]])
        if i >= 1:
            s1_Bev(i - 1)
    dump("d_U", U.rearrange("p a b -> p (a b)"), [128, 32 * 1024], BF16, flat(BU))
    if stop_after <= 1:
        return finish()

    BTAB = [Buf("tabc"), Buf("tabs")]
    BABT = [[Buf("abt%d%d" % (i, j)) for j in range(2)] for i in range(2)]
    BYF = [[Buf("yf%d_%d" % (g, kb)) for kb in range(2)] for g in range(8)]
    YFT = carve(YFT_OFF, [8, NOWN], BF16, flat(BYF))
    TABC = carve(R3, [32, 512], BF16, BTAB[0])
    TABS = carve(R3 + 32 * KB, [32, 512], BF16, BTAB[1])
    ABT = [[carve(R3 + 64 * KB + (2 * i + j) * KB, [512], BF16, BABT[i][j]) for j in range(2)] for i in range(2)]
    for kb in range(2):
        for (tab, srcd, Bt, nm) in ((TABC, dftC, BTAB[0], "tabc"), (TABS, dftS, BTAB[1], "tabs")):
            for hf in range(2):
                src = srcd[hf * 2048:(hf + 1) * 2048, kb * 512:(kb + 1) * 512].rearrange("(t p) k -> p t k", p=128)
                P.dma("sp", I("dma_start", out=tab[:, hf * 16:(hf + 1) * 16, :], in_=src), writes=[Bt], sem=nm)
        for g in range(8):
            i = g % 2
            ba, bb_, by = 0 + i, 2 + i, 4 + i
            ub = [BU[t][g // 4] for t in range(32)]
            fns = [I("matmul", ps(ba), lhsT=U[:, t, g * 128:(g + 1) * 128], rhs=TABC[:, t, :], start=(t == 0), stop=(t == 31)) for t in range(32)]
            P.op("pe", fns, reads=ub + [BTAB[0]], writes=[PSB[ba]])
            fns = [I("matmul", ps(bb_), lhsT=U[:, t, g * 128:(g + 1) * 128], rhs=TABS[:, t, :], start=(t == 0), stop=(t == 31)) for t in range(32)]
            P.op("pe", fns, reads=ub + [BTAB[1]], writes=[PSB[bb_]])
            P.op("act", I("activation", out=ABT[i][0], in_=ps(ba), func=AF.Identity), reads=[PSB[ba]], writes=[BABT[i][0]])
            P.op("dve", I("tensor_copy", out=ABT[i][1], in_=ps(bb_)), reads=[PSB[bb_]], writes=[BABT[i][1]])
            fns = [I("matmul", ps(by), lhsT=chC, rhs=ABT[i][0], start=True, stop=False),
                   I("matmul", ps(by), lhsT=chS, rhs=ABT[i][1], start=False, stop=True)]
            P.op("pe", fns, reads=[BABT[i][0], BABT[i][1], Bc], writes=[PSB[by]])
            if i == 0:
                P.op("act", I("activation", out=YFT[:, g, kb * 512:(kb + 1) * 512], in_=ps(by), func=AF.Identity), reads=[PSB[by]], writes=[BYF[g][kb]])
            else:
                P.op("dve", I("tensor_copy", out=YFT[:, g, kb * 512:(kb + 1) * 512], in_=ps(by)), reads=[PSB[by]], writes=[BYF[g][kb]])
    dump("d_YFT", YFT.rearrange("p a b -> p (a b)"), [128, 8 * 1024], BF16, flat(BYF))
    if stop_after <= 2:
        return finish()

    BHTO = [[Buf("hto%d_%d" % (t, g)) for g in range(4)] for t in range(8)]
    HTO = carve(R1, [16, NOWN], BF16, flat(BHTO))
    BKT = [Buf("kt%d" % t) for t in range(34)]
    BVS = [Buf("vs%d" % t) for t in range(34)]
    KT = carve(R2, [4, 4352], BF16, BKT)
    VS = carve(R2 + 34 * KB, [34, 512], BF16, BVS)
    BWKV = [Buf("wkv0"), Buf("wkv1")]
    WKV = carve(R3, [16, 1024], BF16, BWKV)
    for h in range(2):
        src = w_in[:, K_OFF + h * 512:K_OFF + (h + 1) * 512].rearrange("(c p) f -> p c f", p=128)
        P.dma("pool", I("dma_start", out=WKV[:, :, h * 512:(h + 1) * 512], in_=src), writes=[BWKV[h]], sem="wkv%d" % h)
    o3 = R3 + 32 * KB
    BXS = [Buf("xs0b"), Buf("xs1b")]
    BXN = [Buf("xn0b"), Buf("xn1b")]
    BHT = [[Buf("htb%d_%d" % (s, g)) for g in range(4)] for s in range(2)]
    XS = [carve(o3 + i * 8 * KB, [D], F32, BXS[i]) for i in range(2)]
    XN = [carve(o3 + 16 * KB + i * 4 * KB, [D], BF16, BXN[i]) for i in range(2)]
    HT = [carve(o3 + 24 * KB + i * 4 * KB, [16, 128], BF16, BHT[i]) for i in range(2)]
    o3 += 32 * KB
    BSQ, BKN, BT12, BT34, BKR, BSSK = Buf("sq"), Buf("kn"), Buf("t12"), Buf("t34"), Buf("kr"), Buf("ssk")
    BCS = [Buf("cs0"), Buf("cs1")]
    SQ = carve(o3, [512], F32, BSQ)
    KN = carve(o3 + 2 * KB, [512], F32, BKN)
    T1 = carve(o3 + 4 * KB, [256], F32, BT12)
    T2 = carve(o3 + 5 * KB, [256], F32, BT12)
    T3 = carve(o3 + 6 * KB, [256], F32, BT34)
    T4 = carve(o3 + 7 * KB, [256], F32, BT34)
    KR = carve(o3 + 8 * KB, [512], BF16, BKR)
    CS = [carve(o3 + 9 * KB + i * 512, [128], F32, BCS[i]) for i in range(2)]
    SSK = carve(o3 + 10 * KB, [16], F32, BSSK)

    def qk_norm_rope(psbank, nw_bc, rope_slot, out_bf):
        Bp = PSB[psbank]
        h4 = lambda ap: ap.rearrange("p (h d) -> p h d", h=4)
        P.op("act", I("activation", out=SQ, in_=ps(psbank), func=AF.Square), reads=[Bp], writes=[BSQ])
        P.op("dve", I("tensor_reduce", out=SSK[:, 0:4], in_=h4(SQ), axis=AX.X, op=ALU.add), reads=[BSQ], writes=[BSSK])
        P.op("act", I("activation", out=SSK[:, 4:8], in_=SSK[:, 0:4], func=AF.Sqrt, scale=1.0 / 128, bias=EPS), reads=[BSSK], writes=[BSSK])
        P.op("dve", I("reciprocal", out=SSK[:, 8:12], in_=SSK[:, 4:8]), reads=[BSSK], writes=[BSSK])
        P.op("dve", I("tensor_tensor", out=h4(KN), in0=h4(ps(psbank)), in1=nw_bc.unsqueeze(1).broadcast_to([128, 4, 128]), op=ALU.mult),
             reads=[Bp, Bc], writes=[BKN])
        rbc = SSK[:, 8:12].unsqueeze(2).broadcast_to([128, 4, 128])
        if rope_slot is None:
            P.op("dve", I("tensor_tensor", out=h4(out_bf), in0=h4(KN), in1=rbc, op=ALU.mult), reads=[BKN, BSSK], writes=[BKR])
            return
        P.op("dve", I("tensor_tensor", out=h4(KN), in0=h4(KN), in1=rbc, op=ALU.mult), reads=[BKN, BSSK], writes=[BKN])
        kn5 = KN.rearrange("p (h x y f) -> p h x y f", h=4, x=2, y=2)
        xa, xb = kn5[:, :, :, 0, :], kn5[:, :, :, 1, :]
        o5 = out_bf.rearrange("p (h x y f) -> p h x y f", h=4, x=2, y=2)
        oa, ob = o5[:, :, :, 0, :], o5[:, :, :, 1, :]
        cs = CS[rope_slot]
        cb = cs[:, 0:64].rearrange("p (x f) -> p x f", x=2).unsqueeze(1).broadcast_to([128, 4, 2, 32])
        sb = cs[:, 64:128].rearrange("p (x f) -> p x f", x=2).unsqueeze(1).broadcast_to([128, 4, 2, 32])
        v4 = lambda ap: ap.rearrange("p (h x f) -> p h x f", h=4, x=2)
        Bcs = BCS[rope_slot]
        P.op("dve", [I("tensor_tensor", out=v4(T1), in0=xa, in1=cb, op=ALU.mult), I("tensor_tensor", out=v4(T2), in0=xb, in1=sb, op=ALU.mult)],
             reads=[BKN, Bcs], writes=[BT12])
        P.op("pool", [I("tensor_tensor", out=v4(T3), in0=xb, in1=cb, op=ALU.mult), I("tensor_tensor", out=v4(T4), in0=xa, in1=sb, op=ALU.mult)],
             reads=[BKN, Bcs], writes=[BT34])
        P.op("dve", I("tensor_tensor", out=oa, in0=v4(T1), in1=v4(T2), op=ALU.subtract), reads=[BT12], writes=[BKR])
        P.op("pool", I("tensor_tensor", out=ob, in0=v4(T3), in1=v4(T4), op=ALU.add), reads=[BT34, BKR], writes=[BKR])

    def load_cs(slot, t):
        P.dma("sp", I("dma_start", out=CS[slot][:, 0:64], in_=cosA[t * 128:(t + 1) * 128, :]), writes=[BCS[slot]], sem="cs%d" % slot)
        P.dma("sp", I("dma_start", out=CS[slot][:, 64:128], in_=sinA[t * 128:(t + 1) * 128, :]), writes=[BCS[slot]], sem="cs%d" % slot)

    def s3_tile(t):
        s = t % 2
        if t < 8:
            return (lambda t: lambda c: HTO[:, c, t * 128:(t + 1) * 128])(t), BHTO[t]
        return (lambda s: lambda c: HT[s][:, c, :])(s), BHT[s]

    def s3_rows(t):
        return xall[t * 128:(t + 1) * 128, :] if t < 32 else ctxb[(t - 32) * 128:(t - 31) * 128, :]

    def s3_A(t):
        s = t % 2
        dst_fn, dbufs = s3_tile(t)
        nt_compute(0 if t < 32 else 1, XS[s], XN[s], s, dst_fn, dbufs, BXS[s], BXN[s])

    def s3_B(t):
        s = t % 2
        dst_fn, dbufs = s3_tile(t)
        bkk, bkv = 2 + s, 4 + s
        fns = [I("matmul", ps(bkk), lhsT=dst_fn(c), rhs=WKV[:, c, 0:512], start=(c == 0), stop=(c == 15)) for c in range(16)]
        P.op("pe", fns, reads=dbufs + [BWKV[0]], writes=[PSB[bkk]])
        fns = [I("matmul", ps(bkv), lhsT=dst_fn(c), rhs=WKV[:, c, 512:1024], start=(c == 0), stop=(c == 15)) for c in range(16)]
        P.op("pe", fns, reads=dbufs + [BWKV[1]], writes=[PSB[bkv]])

    def s3_Bev(t):
        bkv = 4 + t % 2
        P.op("act", I("activation", out=VS[:, t, :], in_=ps(bkv), func=AF.Identity), reads=[PSB[bkv]], writes=[BVS[t]])

    def s3_C(t):
        s = t % 2
        qk_norm_rope(2 + s, knw_bc, s if t < 32 else None, KR)
        bt = 6 + s
        pv = psb(bt)[:, 0:512]
        fns = [I("transpose", pv[:, h * 128:(h + 1) * 128], KR[:, h * 128:(h + 1) * 128], identb) for h in range(4)]
        P.op("pe", fns, reads=[BKR, Bc], writes=[PSB[bt]])
        P.op("dve", I("tensor_copy", out=KT[:, :, t * 128:(t + 1) * 128], in_=pv.rearrange("p (h d) -> p h d", h=4)), reads=[PSB[bt]], writes=[BKT[t]])

    nt_load(s3_rows(0), XS[0], BXS[0])
    load_cs(0, 0)
    load_cs(1, 1)
    for i in range(36):
        if i + 1 < 34:
            nt_load(s3_rows(i + 1), XS[(i + 1) % 2], BXS[(i + 1) % 2])
        bm = P.rec(s3_B, i - 1) if 0 <= i - 1 < 34 else []
        a = P.rec(s3_A, i) if i < 34 else []
        c = P.rec(s3_C, i - 2) if 0 <= i - 2 < 34 else []
        P.play([bm[0:1]])
        P.play([a[0:9], c[0:9]])
        P.play([bm[1:2]])
        P.play([a[9:], c[9:]])
        if 0 <= i - 1 < 34:
            s3_Bev(i - 1)
        if 0 <= i - 2 < 34 and i < 32:
            load_cs(i % 2, i)
    dump("d_KT", KT.rearrange("p a b -> p (a b)"), [128, 4 * 4352], BF16, BKT)
    dump("d_VS", VS.rearrange("p a b -> p (a b)"), [128, 34 * 512], BF16, BVS)
    dump("d_HTO", HTO.rearrange("p a b -> p (a b)"), [128, 16 * 1024], BF16, flat(BHTO))
    if stop_after <= 3:
        return finish()

    BQT = [[Buf("qt%d_%d" % (qb, t)) for t in range(8)] for qb in range(4)]
    QT = carve(R3, [16, NOWN], BF16, flat(BQT))
    BWQ = [Buf("wq0"), Buf("wq1")]
    WQ = [carve(R3 + 32 * KB + i * 16 * KB, [16, 512], BF16, BWQ[i]) for i in range(2)]
    def wq_load(qblk):
        src = w_in[:, Q_OFF + qblk * 512:Q_OFF + (qblk + 1) * 512].rearrange("(c p) f -> p c f", p=128)
        P.dma("pool", I("dma_start", out=WQ[qblk % 2], in_=src), writes=[BWQ[qblk % 2]], sem="wq%d" % (qblk % 2))

    wq_load(0)
    wq_load(1)
    its = [(qblk, t) for qblk in range(4) for t in range(8)]

    def s4_mm(qblk, t):
        bq = 2 + (t % 2)
        fns = [I("matmul", ps(bq), lhsT=HTO[:, c, t * 128:(t + 1) * 128], rhs=WQ[qblk % 2][:, c, :], start=(c == 0), stop=(c == 15)) for c in range(16)]
        P.op("pe", fns, reads=BHTO[t] + [BWQ[qblk % 2]], writes=[PSB[bq]])

    s4_mm(*its[0])
    for k, (qblk, t) in enumerate(its):
        if k + 1 < len(its):
            nq, nt = its[k + 1]
            if nt == 0 and nq + 1 < 4:
                pass
            s4_mm(nq, nt)
        if t == 7 and qblk + 2 < 4:
            wq_load(qblk + 2)
        cs_s = t % 2
        load_cs(cs_s, t)
        bq = 2 + (t % 2)
        qk_norm_rope(bq, qnw_bc, cs_s, KR)
        bt = 6 + (t % 2)
        pv = psb(bt)[:, 0:512]
        fns = [I("transpose", pv[:, h * 128:(h + 1) * 128], KR[:, h * 128:(h + 1) * 128], identb) for h in range(4)]
        P.op("pe", fns, reads=[BKR, Bc], writes=[PSB[bt]])
        P.op("dve", I("tensor_copy", out=QT[:, 4 * qblk:4 * qblk + 4, t * 128:(t + 1) * 128], in_=pv.rearrange("p (h d) -> p h d", h=4)),
             reads=[PSB[bt]], writes=[BQT[qblk][t]])
    dump("d_QT", QT.rearrange("p a b -> p (a b)"), [128, 16 * 1024], BF16, flat(BQT))
    if stop_after <= 4:
        return finish()

    BYA = [[Buf("ya%d_%d" % (h, qb)) for qb in range(2)] for h in range(16)]
    YAT = carve(R1, [16, NOWN], BF16, flat(BYA))
    o5_ = R3 + 32 * KB
    BPT = [Buf("pt%d" % i) for i in range(4)]
    BRD = [Buf("rd0"), Buf("rd1")]
    PT = [carve(o5_ + i * KB, [512], BF16, BPT[i]) for i in range(4)]
    RD = [carve(o5_ + 4 * KB + i * 2 * KB, [512], F32, BRD[i]) for i in range(2)]
    SCALE = 128.0 ** -0.5
    it = 0
    pend_a = mod_cols(3 * D, 2 * D, R3 + 40 * KB, 7, 64, False, None, defer=True)

    def mod2_finish():
        P.op("dve", I("tensor_tensor", out=modB, in0=ps(7)[:, 64:128].rearrange("p (a b) -> p a b", b=2),
                      in1=modbT[:, 48:80].unsqueeze(2).broadcast_to([128, 32, 2]), op=ALU.add), reads=[PSB[7], Bc], writes=[Bmod])
        P.op("dve", I("tensor_scalar", out=S2, in0=modB[:, 16:32, 0], scalar1=1.0, scalar2=None, op0=ALU.add), reads=[Bmod], writes=[Bmod])
        P.op("dve", I("tensor_tensor", out=S2, in0=S2, in1=n2T, op=ALU.mult), reads=[Bmod, Bc], writes=[Bmod])
        P.op("dve", I("tensor_copy", out=B2, in_=modB[:, 0:16, 0]), reads=[Bmod], writes=[Bmod])

    for h in range(16):
        kvh = h // 4
        for qb in range(2):
            par = it % 2
            it += 1
            if pend_a:
                pend_a.pop(0)()
                if not pend_a:
                    mod2_finish()
            bo, bd = 3 + par, 5 + par
            qbufs = [BQT[h // 4][4 * qb + j] for j in range(4)]
            qrhs = QT[:, h, qb * 512:(qb + 1) * 512]

            def emit_s(kt):
                bs = kt % 3
                P.op("pe", I("matmul", ps(bs), lhsT=KT[:, kvh, kt * 128:(kt + 1) * 128], rhs=qrhs, start=True, stop=True),
                     reads=[BKT[kt]] + qbufs, writes=[PSB[bs]])

            emit_s(0)
            emit_s(1)
            for kt in range(34):
                if kt + 2 < 34:
                    emit_s(kt + 2)
                bs = kt % 3
                pi = kt % 4
                P.op("act", I("activation", out=PT[pi], in_=ps(bs), func=AF.Exp, scale=SCALE), reads=[PSB[bs]], writes=[BPT[pi]])
                fns = [I("matmul", ps(bo), lhsT=VS[:, kt, kvh * 128:(kvh + 1) * 128], rhs=PT[pi], start=(kt == 0), stop=(kt == 33)),
                       I("matmul", ps(bd), lhsT=onesb, rhs=PT[pi], start=(kt == 0), stop=(kt == 33))]
                P.op("pe", fns, reads=[BVS[kt], BPT[pi], Bc], writes=[PSB[bo], PSB[bd]])
            P.op("dve", I("reciprocal", out=RD[par], in_=ps(bd)), reads=[PSB[bd]], writes=[BRD[par]])
            P.op("dve", I("tensor_tensor", out=YAT[:, h, qb * 512:(qb + 1) * 512], in0=ps(bo), in1=RD[par], op=ALU.mult),
                 reads=[PSB[bo], BRD[par]], writes=[BYA[h][qb]])
    dump("d_YAT", YAT.rearrange("p a b -> p (a b)"), [128, 16 * 1024], BF16, flat(BYA))
    if stop_after <= 5:
        return finish()

    BHTO = [[Buf("hto2_%d_%d" % (t, g)) for g in range(4)] for t in range(8)]
    HTO = carve(R2, [16, NOWN], BF16, flat(BHTO))
    BYT = [[Buf("yt%d_%d" % (dc, tb)) for tb in range(2)] for dc in range(16)]
    YT = carve(R2 + 32 * KB, [16, NOWN], BF16, flat(BYT))
    o6 = R3 + 56 * KB
    BXS6, BXN6 = [Buf("xs6"), Buf("xs6b")], [Buf("xn6"), Buf("xn6b")]
    XS6 = [carve(o6, [D], F32, BXS6[0]), carve(R3 + 28 * KB, [D], F32, BXS6[1])]
    XN6 = [carve(o6 + 8 * KB, [D], BF16, BXN6[0]), carve(R3 + 36 * KB, [D], BF16, BXN6[1])]

    def s6_A(t):
        i = t % 2
        nt_compute(0, XS6[i], XN6[i], i, (lambda t: lambda c: HTO[:, c, t * 128:(t + 1) * 128])(t), BHTO[t], BXS6[i], BXN6[i], bank0=2 * i)

    for t in range(0, 8, 2):
        for i in range(2):
            nt_load(xall[(t + i) * 128:(t + i + 1) * 128, :], XS6[i], BXS6[i])
        P.play([P.rec(s6_A, t), P.rec(s6_A, t + 1)])
    BWS = [Buf("ws0"), Buf("ws1")]
    WS = [carve(R3 + i * 28 * KB, [56, 256], BF16, BWS[i]) for i in range(2)]
    BSG = [Buf("sg%d" % i) for i in range(4)]
    SG = [carve(o6 + 12 * KB + i * 2 * KB, [512], F32, BSG[i]) for i in range(4)]
    step = 0
    def ws_load(cb):
        s = cb % 2
        c0 = cb * 256
        srcs = [(w_fo[:, c0:c0 + 256], 0, 8), (w_ao[:, c0:c0 + 256], 8, 16),
                (w_in[:, GF_OFF + c0:GF_OFF + c0 + 256], 24, 16), (w_in[:, GA_OFF + c0:GA_OFF + c0 + 256], 40, 16)]
        for (srcd, k0, nk) in srcs:
            src = srcd.rearrange("(c p) f -> p c f", p=128)
            P.dma("pool", I("dma_start", out=WS[s][:, k0:k0 + nk, :], in_=src), writes=[BWS[s]], sem="ws%d" % s)

    ws_load(0)
    for cb in range(8):
        s = cb % 2
        if cb + 1 < 8:
            ws_load(cb + 1)
        for cc in range(2):
            dc = 2 * cb + cc
            for tb in range(2):
                par = step % 2
                step += 1
                b_fo, b_ao, b_gf, b_ga = 4 * par, 4 * par + 1, 4 * par + 2, 4 * par + 3
                tsl = slice(tb * 512, (tb + 1) * 512)
                csl = slice(cc * 128, (cc + 1) * 128)
                fns = [I("matmul", ps(b_fo), lhsT=WS[s][:, k, csl], rhs=YFT[:, k, tsl], start=(k == 0), stop=(k == 7)) for k in range(8)]
                P.op("pe", fns, reads=[BWS[s]] + [BYF[g][tb] for g in range(8)], writes=[PSB[b_fo]])
                fns = [I("matmul", ps(b_ao), lhsT=WS[s][:, 8 + k, csl], rhs=YAT[:, k, tsl], start=(k == 0), stop=(k == 15)) for k in range(16)]
                P.op("pe", fns, reads=[BWS[s]] + [BYA[h][tb] for h in range(16)], writes=[PSB[b_ao]])
                hbufs = [b for t in range(4 * tb, 4 * tb + 4) for b in BHTO[t]]
                fns = [I("matmul", ps(b_gf), lhsT=WS[s][:, 24 + k, csl], rhs=HTO[:, k, tsl], start=(k == 0), stop=(k == 15)) for k in range(16)]
                P.op("pe", fns, reads=[BWS[s]] + hbufs, writes=[PSB[b_gf]])
                fns = [I("matmul", ps(b_ga), lhsT=WS[s][:, 40 + k, csl], rhs=HTO[:, k, tsl], start=(k == 0), stop=(k == 15)) for k in range(16)]
                P.op("pe", fns, reads=[BWS[s]] + hbufs, writes=[PSB[b_ga]])
                P.op("act", I("activation", out=SG[0], in_=ps(b_gf), func=AF.Sigmoid), reads=[PSB[b_gf]], writes=[BSG[0]])
                P.op("act", I("activation", out=SG[1], in_=ps(b_ga), func=AF.Sigmoid), reads=[PSB[b_ga]], writes=[BSG[1]])
                P.op("dve", I("tensor_tensor", out=SG[2], in0=ps(b_fo), in1=SG[0], op=ALU.mult), reads=[PSB[b_fo], BSG[0]], writes=[BSG[2]])
                P.op("dve", I("tensor_tensor", out=SG[3], in0=ps(b_ao), in1=SG[1], op=ALU.mult), reads=[PSB[b_ao], BSG[1]], writes=[BSG[3]])
                P.op("pool", I("tensor_tensor", out=YT[:, dc, tsl], in0=SG[2], in1=SG[3], op=ALU.add), reads=[BSG[2], BSG[3]], writes=[BYT[dc][tb]])
    dump("d_YT", YT.rearrange("p a b -> p (a b)"), [128, 16 * 1024], BF16, flat(BYT))
    if stop_after <= 6:
        return finish()

    BG1 = Buf("g1")
    G1 = carve(12 * KB, [D], F32, BG1)
    P.dma("sp", I("dma_start", out=G1, in_=mod_b[0:1, 2 * D:3 * D].partition_broadcast(128)), writes=[BG1], sem="g1")

    def evac_g1(j, bk):
        P.op("dve", I("tensor_tensor", out=G1[:, j * 256:(j + 1) * 256], in0=ps(bk)[:, 0:256], in1=G1[:, j * 256:(j + 1) * 256], op=ALU.add),
             reads=[PSB[bk]], writes=[BG1])

    mod_cols(2 * D, D, R1, 0, 0, True, evac_g1)
    dump("d_G1", G1, [128, D], F32, [BG1])

    BX1 = [[Buf("x1_%d_%d" % (t, db)) for db in range(4)] for t in range(8)]
    X1 = carve(R3, [8, D], F32, flat(BX1))
    for t in range(8):
        P.dma("sp", I("dma_start", out=X1[:, t, :], in_=xall[t * 128:(t + 1) * 128, :]), writes=BX1[t], sem="x1_%d" % t)
    BWO = [Buf("wo0"), Buf("wo1")]
    WO = [carve(R1 + 16 * KB + i * 16 * KB, [16, 512], BF16, BWO[i]) for i in range(2)]
    BTMP = [Buf("tmp0"), Buf("tmp1")]
    TMP = [carve(R2 + 16 * KB + i * 2 * KB, [512], F32, BTMP[i]) for i in range(2)]
    def wo_load(db):
        src = w_o[:, db * 512:(db + 1) * 512].rearrange("(c p) f -> p c f", p=128)
        P.dma("pool", I("dma_start", out=WO[db % 2], in_=src), writes=[BWO[db % 2]], sem="wo%d" % (db % 2))

    BG2F, BG2 = Buf("g2f"), Buf("g2")
    G2F = carve(36 * KB, [D], F32, BG2F)
    G2 = carve(124 * KB, [D], BF16, BG2)
    P.dma("sp", I("dma_start", out=G2F, in_=mod_b[0:1, 5 * D:6 * D].partition_broadcast(128)), writes=[BG2F], sem="g2f")

    def evac_g2(j, bk):
        P.op("dve", I("tensor_tensor", out=G2[:, j * 256:(j + 1) * 256], in0=ps(bk)[:, 0:256], in1=G2F[:, j * 256:(j + 1) * 256], op=ALU.add),
             reads=[PSB[bk], BG2F], writes=[BG2])

    pending = mod_cols(5 * D, D, 20 * KB, 0, 0, True, evac_g2, defer=True)
    wo_load(0)
    for db in range(4):
        s = db % 2
        if db + 1 < 4:
            wo_load(db + 1)
        for t in range(8):
            if pending:
                pending.pop(0)()
            bk = 2 + (t % 4)
            i = t % 2
            fns = [I("matmul", ps(bk), lhsT=YT[:, k, t * 128:(t + 1) * 128], rhs=WO[s][:, k, :], start=(k == 0), stop=(k == 15)) for k in range(16)]
            P.op("pe", fns, reads=[BYT[k][t // 4] for k in range(16)] + [BWO[s]], writes=[PSB[bk]])
            P.op("dve", I("tensor_tensor", out=TMP[i], in0=ps(bk), in1=G1[:, db * 512:(db + 1) * 512], op=ALU.mult),
                 reads=[PSB[bk], BG1], writes=[BTMP[i]])
            xv = X1[:, t, db * 512:(db + 1) * 512]
            P.op("dve", I("tensor_tensor", out=xv, in0=xv, in1=TMP[i], op=ALU.add), reads=[BTMP[i]], writes=[BX1[t][db]])
    if debug:
        d = nc.dram_tensor("d_X1", [NOWN, D], F32, kind="ExternalOutput").ap()
        b = Buf("dbg_x1")
        for t in range(8):
            P.dma("sp", I("dma_start", out=d[t * 128:(t + 1) * 128, :], in_=X1[:, t, :]), reads=BX1[t], writes=[b], sem="dbg")
        DBGB.append(b)
    if stop_after <= 7:
        return finish()

    while pending:
        pending.pop(0)()
    dump("d_G2", G2, [128, D], BF16, [BG2])
    dump("d_S2", S2, [128, 16], F32, [Bmod])
    if stop_after <= 7.5:
        return finish()

    BH2 = [[Buf("h2_%d_%d" % (t, g)) for g in range(4)] for t in range(8)]
    H2T = carve(R1, [16, NOWN], BF16, flat(BH2))
    o8 = R2
    BRW, BJ, BSC, BRT, BSM = Buf("rw"), Buf("junk"), Buf("sc"), Buf("rt"), Buf("sm")
    BXN2 = [Buf("xn2_0"), Buf("xn2_1")]
    BH2F = [[Buf("h2f%d_%d" % (i, g)) for g in range(4)] for i in range(2)]
    RW = carve(o8, [16, NE], F32, BRW)
    XN2 = [carve(o8 + 4 * KB + i * 8 * KB, [D], F32, BXN2[i]) for i in range(2)]
    H2F = [carve(o8 + 20 * KB + i * 8 * KB, [16, 128], F32, BH2F[i]) for i in range(2)]
    JUNK = carve(o8 + 36 * KB, [D], BF16, BJ)
    SC = carve(o8 + 40 * KB, [8, NE], F32, BSC)
    RT = [carve(o8 + 42 * KB + i * 2 * KB, [8, NE], F32, BRT) for i in range(4)]
    SM = carve(o8 + 50 * KB, [256], F32, BSM)
    P.dma("sp", I("dma_start", out=RW, in_=router_w.rearrange("(c p) f -> p c f", p=128)), writes=[BRW], sem="rw")

    def s8_A(t):
        s = t % 4
        stt = stat[s]
        xn2 = XN2[t % 2]
        P.op("pool", I("memset", stt[:, 0:1], 0.0), writes=[Bstat[s]])
        P.op("act", I("activation", out=JUNK, in_=X1[:, t, :], func=AF.Square, accum_out=stt[:, 0:1]), reads=BX1[t], writes=[BJ, Bstat[s]])
        P.op("act", I("activation", out=stt[:, 1:2], in_=stt[:, 0:1], func=AF.Sqrt, scale=1.0 / D, bias=EPS), reads=[Bstat[s]], writes=[Bstat[s]])
        P.op("dve", I("reciprocal", out=stt[:, 2:3], in_=stt[:, 1:2]), reads=[Bstat[s]], writes=[Bstat[s]])
        P.op("dve", I("tensor_scalar", out=xn2, in0=X1[:, t, :], scalar1=stt[:, 2:3], scalar2=None, op0=ALU.mult), reads=BX1[t] + [Bstat[s]], writes=[BXN2[t % 2]])

    def s8_B(t):
        xn2, h2f = XN2[t % 2], H2F[t % 2]
        for g in range(4):
            bk = g
            fns = [I("transpose", ps(bk)[:, j * 128:(j + 1) * 128], xn2[:, (4 * g + j) * 128:(4 * g + j + 1) * 128], identf) for j in range(4)]
            P.op("pe", fns, reads=[BXN2[t % 2], Bc], writes=[PSB[bk]])
            fns = [I("activation", out=H2T[:, 4 * g + j, t * 128:(t + 1) * 128], in_=ps(bk)[:, j * 128:(j + 1) * 128], func=AF.Identity,
                     scale=S2[:, 4 * g + j:4 * g + j + 1], bias=B2[:, 4 * g + j:4 * g + j + 1]) for j in range(4)]
            P.op("act", fns, reads=[PSB[bk], Bmod], writes=[BH2[t][g]])
            fns = [I("tensor_scalar", out=h2f[:, 4 * g + j, :], in0=ps(bk)[:, j * 128:(j + 1) * 128], scalar1=S2[:, 4 * g + j:4 * g + j + 1],
                     scalar2=B2[:, 4 * g + j:4 * g + j + 1], op0=ALU.mult, op1=ALU.add) for j in range(4)]
            P.op("dve", fns, reads=[PSB[bk], Bmod], writes=[BH2F[t % 2][g]])

    def s8_C(t):
        h2f = H2F[t % 2]
        br = 4 + (t % 2)
        fns = [I("matmul", ps(br)[:, 0:NE], lhsT=h2f[:, c, :], rhs=RW[:, c, :], start=(c == 0), stop=(c == 15)) for c in range(16)]
        P.op("pe", fns, reads=BH2F[t % 2] + [BRW], writes=[PSB[br]])

    def s8_Cev(t):
        br = 4 + (t % 2)
        P.op("act", I("activation", out=SC[:, t, :], in_=ps(br)[:, 0:NE], func=AF.Sigmoid), reads=[PSB[br]], writes=[BSC])

    for i in range(10):
        ch = []
        if i < 8:
            ch.append(P.rec(s8_A, i))
        if 0 <= i - 1 < 8:
            ch.append(P.rec(s8_B, i - 1))
        if 0 <= i - 2 < 8:
            ch.insert(0, P.rec(s8_C, i - 2))
        P.play(ch)
        if 0 <= i - 2 < 8:
            s8_Cev(i - 2)
    if stop_after <= 7.7:
        dump("d_H2T", H2T.rearrange("p a b -> p (a b)"), [128, 16 * 1024], BF16, flat(BH2))
        dump("d_SC", SC.rearrange("p a b -> p (a b)"), [128, 8 * NE], F32, [BSC])
        return finish()
    BI, M1, GS, PEN = RT[0], SM[:, 0:64], SM[:, 64:128], SM[:, 128:192]
    TOP = SM[:, 192:200]
    THR = SM[:, 200:216]
    WS_ = SM[:, 216:232]
    g8 = lambda ap: ap.rearrange("p t (g k) -> p (t g) k", k=8)
    bi4, e4 = g8(BI), g8(RT[1])
    P.op("dve", I("tensor_tensor", out=BI, in0=SC, in1=rb_bc.unsqueeze(1).broadcast_to([128, 8, NE]), op=ALU.add), reads=[BSC, Bc], writes=[BRT])
    P.op("dve", I("tensor_reduce", out=M1, in_=bi4, axis=AX.X, op=ALU.max), reads=[BRT], writes=[BSM])
    P.op("dve", I("tensor_tensor", out=e4, in0=bi4, in1=M1.unsqueeze(2).broadcast_to([128, 64, 8]), op=ALU.is_equal), reads=[BRT, BSM], writes=[BRT])
    P.op("dve", I("scalar_tensor_tensor", out=e4, in0=e4, scalar=-1e30, in1=bi4, op0=ALU.mult, op1=ALU.add), reads=[BRT], writes=[BRT])
    P.op("dve", I("tensor_reduce", out=GS, in_=e4, axis=AX.X, op=ALU.max), reads=[BRT], writes=[BSM])
    P.op("dve", I("tensor_tensor", out=GS, in0=GS, in1=M1, op=ALU.add), reads=[BSM], writes=[BSM])
    for t in range(8):
        P.op("dve", I("max", out=TOP, in_=GS[:, t * 8:(t + 1) * 8]), reads=[BSM], writes=[BSM])
        P.op("dve", I("tensor_copy", out=THR[:, t:t + 1], in_=TOP[:, 3:4]), reads=[BSM], writes=[BSM])
    gs3 = GS.rearrange("p (t g) -> p t g", g=8)
    pen3 = PEN.rearrange("p (t g) -> p t g", g=8)
    P.op("dve", I("tensor_tensor", out=pen3, in0=gs3, in1=THR[:, 0:8].unsqueeze(2).broadcast_to([128, 8, 8]), op=ALU.is_ge), reads=[BSM], writes=[BSM])
    P.op("dve", I("tensor_scalar", out=PEN, in0=PEN, scalar1=1e30, scalar2=-1e30, op0=ALU.mult, op1=ALU.add), reads=[BSM], writes=[BSM])
    MK = RT[2]
    P.op("dve", I("tensor_tensor", out=g8(MK), in0=bi4, in1=PEN.unsqueeze(2).broadcast_to([128, 64, 8]), op=ALU.add), reads=[BRT, BSM], writes=[BRT])
    for t in range(8):
        P.op("dve", I("max", out=TOP, in_=MK[:, t, :]), reads=[BRT, BSM], writes=[BSM])
        P.op("dve", I("tensor_copy", out=THR[:, 8 + t:9 + t], in_=TOP[:, 7:8]), reads=[BSM], writes=[BSM])
    SEL = RT[3]
    P.op("dve", I("tensor_tensor", out=SEL, in0=MK, in1=THR[:, 8:16].unsqueeze(2).broadcast_to([128, 8, NE]), op=ALU.is_ge), reads=[BRT, BSM], writes=[BRT])
    P.op("dve", I("tensor_tensor", out=SEL, in0=SEL, in1=SC, op=ALU.mult), reads=[BRT, BSC], writes=[BRT])
    P.op("dve", I("tensor_reduce", out=WS_[:, 0:8], in_=SEL, axis=AX.X, op=ALU.add), reads=[BRT], writes=[BSM])
    P.op("dve", I("reciprocal", out=WS_[:, 8:16], in_=WS_[:, 0:8]), reads=[BSM], writes=[BSM])
    P.op("dve", I("tensor_tensor", out=wdense, in0=SEL, in1=WS_[:, 8:16].unsqueeze(2).broadcast_to([128, 8, NE]), op=ALU.mult), reads=[BRT, BSM], writes=[Bwd])
    P.op("dve", I("tensor_scalar", out=wdense, in0=wdense, scalar1=2.5, scalar2=None, op0=ALU.mult), reads=[Bwd], writes=[Bwd])
    dump("d_H2T", H2T.rearrange("p a b -> p (a b)"), [128, 16 * 1024], BF16, flat(BH2))
    dump("d_WD", wdense.rearrange("p a b -> p (a b)"), [128, 8 * NE], F32, [Bwd])
    dump("d_SC", SC.rearrange("p a b -> p (a b)"), [128, 8 * NE], F32, [BSC])
    if stop_after <= 8:
        return finish()

    BRING = [Buf("ring%d" % i) for i in range(4)]
    RING = [carve(R2 + i * 16 * KB, [8192], BF16, BRING[i]) for i in range(4)]
    BAT = [[[Buf("at%d_%d_%d" % (p, fc, tb)) for tb in range(2)] for fc in range(4)] for p in range(2)]
    AT = [carve(192 * KB, [4, NOWN], BF16, flat(BAT[0])), carve(16 * KB, [4, NOWN], BF16, flat(BAT[1]))]
    NEXP = NE + 1

    def wsrc(e_, m):
        if e_ < NE:
            return (exp_gate[e_], exp_up[e_], exp_down[e_])[m]
        return (sh_gate, sh_up, sh_down)[m]

    blocks = [(e_, m) for e_ in range(NEXP) for m in range(3)]

    def issue(k):
        e_, m = blocks[k]
        s = k % 4
        nchunk = 16 if m < 2 else 4
        dst = RING[s].rearrange("p (c f) -> p c f", c=nchunk)
        src = wsrc(e_, m).rearrange("(c p) f -> p c f", p=128)
        P.dma("pool", I("dma_start", out=dst, in_=src), writes=[BRING[s]], sem="ring%d" % s)
        if m == 2:
            P.op("pool", I("tensor_tensor", out=dst, in0=dst, in1=G2.unsqueeze(1).broadcast_to([128, 4, D]), op=ALU.mult),
                 reads=[BG2], writes=[BRING[s]])

    for k in range(3):
        issue(k)
    for e_ in range(NEXP):
        par = e_ % 2
        kg, ku, kd = 3 * e_, 3 * e_ + 1, 3 * e_ + 2
        if kg + 3 < len(blocks):
            issue(kg + 3)
        for (kk, is_gate) in ((kg, True), (ku, False)):
            if not is_gate and ku + 3 < len(blocks):
                issue(ku + 3)
            W = RING[kk % 4].rearrange("p (c f) -> p c f", c=16)
            for fc in range(4):
                for tb in range(2):
                    bk = (fc * 2 + tb) % 4
                    fns = [I("matmul", ps(bk), lhsT=W[:, c, fc * 128:(fc + 1) * 128], rhs=H2T[:, c, tb * 512:(tb + 1) * 512],
                             start=(c == 0), stop=(c == 15)) for c in range(16)]
                    P.op("pe", fns, reads=[BRING[kk % 4]] + [b for t in range(4 * tb, 4 * tb + 4) for b in BH2[t]], writes=[PSB[bk]])
                    av = AT[par][:, fc, tb * 512:(tb + 1) * 512]
                    if is_gate:
                        P.op("act", I("activation", out=av, in_=ps(bk), func=AF.Silu), reads=[PSB[bk]], writes=[BAT[par][fc][tb]])
                    else:
                        P.op("dve", I("tensor_tensor", out=av, in0=ps(bk), in1=av, op=ALU.mult), reads=[PSB[bk], BAT[par][fc][tb]], writes=[BAT[par][fc][tb]])
        if kd + 3 < len(blocks):
            issue(kd + 3)
        WD = RING[kd % 4].rearrange("p (c f) -> p c f", c=4)
        for t in range(8):
            for db in range(4):
                bk = 4 + db
                fns = [I("matmul", ps(bk), lhsT=AT[par][:, fc, t * 128:(t + 1) * 128], rhs=WD[:, fc, db * 512:(db + 1) * 512],
                         start=(fc == 0), stop=(fc == 3)) for fc in range(4)]
                P.op("pe", fns, reads=[BRING[kd % 4]] + [BAT[par][fc][t // 4] for fc in range(4)], writes=[PSB[bk]])
                xv = X1[:, t, db * 512:(db + 1) * 512]
                if e_ < NE:
                    P.op("dve", I("scalar_tensor_tensor", out=xv, in0=ps(bk), scalar=wdense[:, t, e_:e_ + 1], in1=xv, op0=ALU.mult, op1=ALU.add),
                         reads=[PSB[bk], Bwd], writes=[BX1[t][db]])
                else:
                    P.op("dve", I("tensor_tensor", out=xv, in0=ps(bk), in1=xv, op=ALU.add), reads=[PSB[bk]], writes=[BX1[t][db]])
    for t in range(8):
        b = Buf("out%d" % t)
        P.dma("sp", I("dma_start", out=yout[t * 128:(t + 1) * 128, :], in_=X1[:, t, :]), reads=BX1[t], writes=[b], sem="out")
        OUTB.append(b)
    return finish()


_CACHE = {}


def _const_tables():
    if "tabs" in _CACHE:
        return _CACHE["tabs"]
    bf = ml_dtypes.bfloat16
    n = np.arange(S, dtype=np.int64)
    tabs = {}
    tabs["identb"] = np.eye(128, dtype=np.float32).astype(bf)
    tabs["identf"] = np.eye(128, dtype=np.float32)
    tabs["onesb"] = np.ones((128, 128), dtype=np.float32).astype(bf)
    c = np.arange(128, dtype=np.int64)
    ang = 2.0 * np.pi * ((c[:, None] * c[None, :]) % 128) / 128.0
    sc = 1.0 / np.sqrt(float(S) * 128.0)
    tabs["chC"] = (np.cos(ang) * sc).astype(np.float32).astype(bf)
    tabs["chS"] = (-np.sin(ang) * sc).astype(np.float32).astype(bf)
    inv = (10000.0 ** (-np.arange(32, dtype=np.float32) / 32.0)).astype(np.float32)
    pos = np.stack([n // 64, n % 64], axis=-1).astype(np.float32)
    a = (pos[:, :, None] * inv[None, None, :]).astype(np.float32)
    tabs["cos"] = np.cos(a).astype(np.float32).reshape(S, 64)
    tabs["sin"] = np.sin(a).astype(np.float32).reshape(S, 64)
    _CACHE["tabs"] = tabs
    return tabs


def _core_order(j):
    own = np.arange(j * NOWN, (j + 1) * NOWN)
    rest = np.concatenate([np.arange(0, j * NOWN), np.arange((j + 1) * NOWN, S)])
    return np.concatenate([own, rest]).astype(np.int64)


def prepare_inputs(inp):
    bf = ml_dtypes.bfloat16
    tabs = _const_tables()
    f32 = lambda a: np.ascontiguousarray(np.asarray(a, dtype=np.float32))
    x = f32(inp["x"])
    c = f32(inp["c"])
    ctx = f32(inp["ctx"])
    c_ctx = f32(inp["c_ctx"])
    shared = {
        "mod_w": f32(inp["mod_w"][0]),
        "mod_bT": f32(inp["mod_b"][0].reshape(96, 128).T),
        "mod_b": f32(inp["mod_b"][0].reshape(1, -1)),
        "norm1T": f32(inp["norm1_w"][0].reshape(16, 128).T),
        "norm2T": f32(inp["norm2_w"][0].reshape(16, 128).T),
        "w_in": f32(inp["w_in"][0]),
        "qnw": f32(inp["q_norm_w"][0].reshape(1, 128)),
        "knw": f32(inp["k_norm_w"][0].reshape(1, 128)),
        "w_fo": f32(inp["w_fourier_out"][0]),
        "w_ao": f32(inp["w_attn_out"][0]),
        "w_o": f32(inp["w_out"][0]),
        "router_w": f32(inp["router_w"][0]),
        "router_b": f32(inp["router_b"][0].reshape(1, NE)),
        "exp_gate": f32(inp["exp_gate"][0]),
        "exp_up": f32(inp["exp_up"][0]),
        "exp_down": f32(inp["exp_down"][0]),
        "sh_gate": f32(inp["shared_gate"][0]),
        "sh_up": f32(inp["shared_up"][0]),
        "sh_down": f32(inp["shared_down"][0]),
        "identb": tabs["identb"], "identf": tabs["identf"], "onesb": tabs["onesb"],
        "chC": tabs["chC"], "chS": tabs["chS"],
    }
    dft = {}
    for j in range(4):
        order = _core_order(j)
        k = np.arange(j * NOWN, (j + 1) * NOWN, dtype=np.int64)
        ang = (2.0 * np.pi / S) * ((order[:, None] * k[None, :]) % S).astype(np.float64)
        dft[j] = (np.cos(ang).astype(np.float32).astype(bf), np.sin(ang).astype(np.float32).astype(bf),
                  np.ascontiguousarray(tabs["cos"][order]), np.ascontiguousarray(tabs["sin"][order]), order)
    in_maps = []
    for core in range(8):
        b, j = core // 4, core % 4
        dC, dS, cs, sn, order = dft[j]
        cond = np.stack([c[b], c_ctx], axis=-1)
        condT = cond.reshape(16, 128, 2).transpose(1, 0, 2).reshape(128, 32)
        m = dict(shared)
        m.update({
            "xall": np.ascontiguousarray(x[b][order]),
            "ctxb": np.ascontiguousarray(ctx[b]),
            "condT": np.ascontiguousarray(condT),
            "cosA": cs, "sinA": sn, "dftC": dC, "dftS": dS,
        })
        in_maps.append(m)
    return in_maps


def kernel(**inputs):
    if "nc" not in _CACHE:
        _CACHE["nc"] = build_program()
    nc = _CACHE["nc"]
    in_maps = prepare_inputs(inputs)
    res = run_bass_kernel_spmd(nc, in_maps, core_ids=list(range(8)))
    out = np.empty((2, S, D), dtype=np.float32)
    for core in range(8):
        b, j = core // 4, core % 4
        out[b, j * NOWN:(j + 1) * NOWN, :] = res.results[core]["yout"]
    return out
```
